# Optimizing a Trainium2 kernel written in Bass

```python
import jax, jax.numpy as jnp
from jax import lax
import numpy as np


D_MODEL = 1024
BATCH = 4
SEQ = 8192
DEPTH = 4

CTX_LEN = 256
GRID_W = 64
N_EVEN = (DEPTH + 1) // 2
N_ODD = DEPTH // 2
EPS = 1e-6

HG_HEADS = 4
HG_DK = 128
HG_DV = 128
HG_W = HG_HEADS * HG_DV
ML_HEADS = 4
ML_DQK = 64
ML_DV = 128
ML_W = ML_HEADS * ML_DV
ML_FGATE_BIAS = 3.0
REC_CHUNK = 64
ROPE_BASE = 10000.0
REC_COLS = (HG_HEADS * HG_DK, HG_HEADS * HG_DK, HG_HEADS * HG_DK, HG_W, HG_W,
            ML_HEADS * ML_DQK, ML_HEADS * ML_DQK, ML_W, ML_W,
            ML_HEADS, ML_HEADS, ML_HEADS, ML_HEADS)
REC_P = sum(REC_COLS)

NA_HEADS = 16
NA_DH = 64
NA_W = NA_HEADS * NA_DH
NA_KR = 8
NA_KC = 16

N_EXPERTS = 32
TOP_K = 4
D_FF = 1024
SWIGLU_LIMIT = 7.0
SWIGLU_ALPHA = 1.702

kernel_name = 'hybrid_hgrn2_mlstm_natten_moe_dit'

F32 = jnp.float32


def rmsnorm(x, g):
    xf = x.astype(F32)
    y = xf * lax.rsqrt(jnp.mean(xf * xf, axis=-1, keepdims=True) + EPS)
    return (y * g.astype(F32)).astype(x.dtype)


def split_cols(p, sizes):
    idx = np.cumsum(np.array(sizes))[:-1].tolist()
    return jnp.split(p, idx, axis=-1)


def to_heads(a, n_heads):
    b, t, _ = a.shape
    return a.reshape(b, t, n_heads, -1).transpose(0, 2, 1, 3)


def from_heads(a):
    b, h, t, d = a.shape
    return a.transpose(0, 2, 1, 3).reshape(b, t, h * d)


def head_norm(o, g):
    return rmsnorm(o, g.reshape(o.shape[1], 1, o.shape[3]))


def axial_rope(x, n_tok):
    d = x.shape[-1]
    half, quarter = d // 2, d // 4
    pos = jnp.arange(n_tok)
    row = (pos // GRID_W).astype(F32)
    col = (pos % GRID_W).astype(F32)
    inv = ROPE_BASE ** (-jnp.arange(quarter, dtype=F32) / quarter)

    def rot(xa, p):
        ang = p[:, None] * inv[None, :]
        cs, sn = jnp.cos(ang), jnp.sin(ang)
        xa = xa.astype(F32)
        x1, x2 = xa[..., :quarter], xa[..., quarter:]
        return jnp.concatenate([x1 * cs - x2 * sn, x2 * cs + x1 * sn], axis=-1)

    out = jnp.concatenate([rot(x[..., :half], row), rot(x[..., half:], col)], axis=-1)
    return out.astype(x.dtype)


def _chunks(a, L):
    b, h, t = a.shape[:3]
    return jnp.moveaxis(a.reshape((b, h, t // L, L) + a.shape[3:]), 2, 0)


def _unchunk(a):
    n, b, h, L = a.shape[:4]
    return jnp.moveaxis(a, 0, 2).reshape((b, h, n * L) + a.shape[4:])


def gla_chunk_scan(inputs, S0):
    q, k, loga, v = inputs
    L = REC_CHUNK
    causal = jnp.tril(jnp.ones((L, L), bool))

    def step(S, blk):
        qb, kb, ab, vb = blk
        b = jnp.cumsum(ab, axis=2)
        diff = b[:, :, :, None, :] - b[:, :, None, :, :]
        decay = jnp.exp(jnp.where(causal[:, :, None], diff, -jnp.inf))
        att = jnp.sum(qb[:, :, :, None, :] * kb[:, :, None, :, :] * decay, axis=-1)
        o = (jnp.einsum('bhts,bhsv->bhtv', att, vb)
             + jnp.einsum('bhtd,bhdv->bhtv', qb * jnp.exp(b), S))
        b_end = b[:, :, -1:, :]
        S = (jnp.exp(b_end[:, :, 0, :, None]) * S
             + jnp.einsum('bhsd,bhsv->bhdv', kb * jnp.exp(b_end - b), vb))
        return S, o

    S, o = lax.scan(step, S0, tuple(_chunks(a, L) for a in (q, k, loga, v)))
    return _unchunk(o), S


def mlstm_chunk_scan(inputs, state0):
    q, k, v, ig, lf = inputs
    L = REC_CHUNK
    causal = jnp.tril(jnp.ones((L, L), bool))

    def step(state, blk):
        C, n, m = state
        qb, kb, vb, ib, fb = blk
        b = jnp.cumsum(fb, axis=-1)
        logw = jnp.where(causal, b[..., :, None] - b[..., None, :] + ib[..., None, :], -jnp.inf)
        log_inter = b + m[..., None]
        m_t = jnp.maximum(log_inter, jnp.max(logw, axis=-1))
        w = jnp.exp(logw - m_t[..., None])
        w_inter = jnp.exp(log_inter - m_t)
        att = jnp.einsum('bhtd,bhsd->bhts', qb, kb) * w
        num = (jnp.einsum('bhts,bhsv->bhtv', att, vb)
               + w_inter[..., None] * jnp.einsum('bhtd,bhdv->bhtv', qb, C))
        den = jnp.sum(att, axis=-1) + w_inter * jnp.einsum('bhtd,bhd->bht', qb, n)
        h = num / jnp.maximum(jnp.abs(den), jnp.exp(-m_t))[..., None]
        b_end = b[..., -1]
        log_k = b_end[..., None] - b + ib
        m_new = jnp.maximum(b_end + m, jnp.max(log_k, axis=-1))
        wk = jnp.exp(log_k - m_new[..., None])
        wc = jnp.exp(b_end + m - m_new)
        C = wc[..., None, None] * C + jnp.einsum('bhs,bhsd,bhsv->bhdv', wk, kb, vb)
        n = wc[..., None] * n + jnp.einsum('bhs,bhsd->bhd', wk, kb)
        return (C, n, m_new), h

    state, h = lax.scan(step, state0, tuple(_chunks(a, L) for a in (q, k, v, ig, lf)))
    return _unchunk(h), state


def bidir_scan(scan_fn, ctx_f, ctx_b, lat_f, lat_b, state0):
    rev = lambda xs: tuple(jnp.flip(a, axis=2) for a in xs)
    oc_f, sc_f = scan_fn(ctx_f, state0)
    o_f, _ = scan_fn(lat_f, sc_f)
    oc_b, sc_b = scan_fn(rev(ctx_b), state0)
    o_b, _ = scan_fn(rev(lat_b), sc_b)
    return o_f + jnp.flip(o_b, axis=2), oc_f + jnp.flip(oc_b, axis=2)


def hgrn_gates(f_pre, lb):
    log_f = jnp.logaddexp(jnp.log(lb), jnp.log1p(-lb) + jax.nn.log_sigmoid(f_pre))
    key = (1.0 - lb) * jax.nn.sigmoid(-f_pre)
    return key, log_f


def rec_streams(p, n_tok, lb, rotate):
    (hq, hf_f, hf_b, hi, hg, mq, mk, mv, mo, mi_f, mf_f, mi_b, mf_b) = split_cols(p.astype(F32), REC_COLS)
    q = to_heads(jax.nn.silu(hq), HG_HEADS) * HG_DK ** -0.5
    k_f, la_f = hgrn_gates(to_heads(hf_f, HG_HEADS), lb)
    k_b, la_b = hgrn_gates(to_heads(hf_b, HG_HEADS), lb)
    v = to_heads(hi, HG_HEADS)
    mq = to_heads(mq, ML_HEADS)
    mk = to_heads(mk, ML_HEADS) * ML_DQK ** -0.5
    if rotate:
        mq = axial_rope(mq, n_tok)
        mk = axial_rope(mk, n_tok)
    mv = to_heads(mv, ML_HEADS)
    gt = lambda a: a.transpose(0, 2, 1)
    ml_f = (mq, mk, mv, gt(mi_f), jax.nn.log_sigmoid(gt(mf_f)))
    ml_b = (mq, mk, mv, gt(mi_b), jax.nn.log_sigmoid(gt(mf_b)))
    return (q, k_f, la_f, v), (q, k_b, la_b, v), ml_f, ml_b, hg, mo


def rec_mixer(h, hc, w_in, b_in, w_out, lb, hg_g, ml_g):
    bsz = h.shape[0]
    lat = rec_streams(h @ w_in + b_in, h.shape[1], lb, True)
    ctx = rec_streams(hc @ w_in + b_in, hc.shape[1], lb, False)
    s0 = jnp.zeros((bsz, HG_HEADS, HG_DK, HG_DV), F32)
    o_hg, oc_hg = bidir_scan(gla_chunk_scan, ctx[0], ctx[1], lat[0], lat[1], s0)
    m0 = (jnp.zeros((bsz, ML_HEADS, ML_DQK, ML_DV), F32),
          jnp.zeros((bsz, ML_HEADS, ML_DQK), F32),
          jnp.zeros((bsz, ML_HEADS), F32))
    o_ml, oc_ml = bidir_scan(mlstm_chunk_scan, ctx[2], ctx[3], lat[2], lat[3], m0)

    def merge(a_hg, a_ml, g_hg, g_ml):
        ya = from_heads(head_norm(a_hg, hg_g)) * jax.nn.silu(g_hg)
        yb = from_heads(head_norm(a_ml, ml_g)) * jax.nn.sigmoid(g_ml)
        return jnp.concatenate([ya, yb], axis=-1).astype(h.dtype) @ w_out

    return merge(o_hg, o_ml, lat[4], lat[5]), merge(oc_hg, oc_ml, ctx[4], ctx[5])


def neighbourhood_attention(q, k, v, kc, vc, rpb):
    bsz, nh, t, dh = q.shape
    rows = t // GRID_W
    kr = min(NA_KR, rows)
    scale = dh ** -0.5
    grid = lambda a: a.reshape(bsz, nh, rows, GRID_W, dh)
    qg, kg, vg = grid(q), grid(k), grid(v)
    cpos = jnp.arange(GRID_W)
    cstart = jnp.clip(cpos - NA_KC // 2, 0, GRID_W - NA_KC)
    col_ok = (cpos[None, :] >= cstart[:, None]) & (cpos[None, :] < cstart[:, None] + NA_KC)
    rel_c = jnp.clip(cpos[None, :] - cpos[:, None], -(NA_KC - 1), NA_KC - 1) + NA_KC - 1
    bias_c = rpb[:, :, rel_c].astype(F32)
    n_loc = kr * GRID_W

    def row_block(r):
        rs = jnp.clip(r - kr // 2, 0, rows - kr)
        kb = lax.dynamic_slice_in_dim(kg, rs, kr, axis=2)
        vb = lax.dynamic_slice_in_dim(vg, rs, kr, axis=2)
        qb = lax.dynamic_index_in_dim(qg, r, axis=2, keepdims=False)
        s_loc = jnp.einsum('bhqd,bhrkd->bhqrk', qb, kb).astype(F32) * scale
        rel_r = rs + jnp.arange(kr) - r + NA_KR - 1
        bias = bias_c[:, rel_r].transpose(0, 2, 1, 3)
        s_loc = jnp.where(col_ok[:, None, :], s_loc + bias[None], -jnp.inf)
        s_loc = s_loc.reshape(bsz, nh, GRID_W, n_loc)
        s_ctx = jnp.einsum('bhqd,bhcd->bhqc', qb, kc).astype(F32) * scale
        p = jax.nn.softmax(jnp.concatenate([s_loc, s_ctx], axis=-1), axis=-1).astype(v.dtype)
        return (jnp.einsum('bhqk,bhkd->bhqd', p[..., :n_loc], vb.reshape(bsz, nh, n_loc, dh))
                + jnp.einsum('bhqc,bhcd->bhqd', p[..., n_loc:], vc))

    out = lax.map(row_block, jnp.arange(rows))
    return out.transpose(1, 2, 0, 3, 4).reshape(bsz, nh, t, dh)


def dense_attention(q, k, v):
    s = jnp.einsum('bhqd,bhkd->bhqk', q, k).astype(F32) * q.shape[-1] ** -0.5
    p = jax.nn.softmax(s, axis=-1).astype(v.dtype)
    return jnp.einsum('bhqk,bhkd->bhqd', p, v)


def na_mixer(h, hc, w_qkv, w_out, q_g, k_g, rpb, with_ctx_out):
    def project(a):
        q, k, v = jnp.split(a @ w_qkv, 3, axis=-1)
        return to_heads(q, NA_HEADS), rmsnorm(to_heads(k, NA_HEADS), k_g), to_heads(v, NA_HEADS)

    q, k, v = project(h)
    qc, kc, vc = project(hc)
    y = from_heads(neighbourhood_attention(rmsnorm(q, q_g), k, v, kc, vc, rpb)) @ w_out
    if not with_ctx_out:
        return y, None
    yc = from_heads(dense_attention(rmsnorm(qc, q_g), kc, vc)) @ w_out
    return y, yc


def moe(h, w_r, b_r, w_gu, b_gu, w_d, b_d):
    logits = (h @ w_r + b_r).astype(F32)
    top_v, top_i = lax.top_k(logits, TOP_K)
    top_w = jax.nn.softmax(top_v, axis=-1)
    comb = jnp.sum(jax.nn.one_hot(top_i, N_EXPERTS, dtype=F32) * top_w[..., None], axis=1)

    def expert(acc, prm):
        wgu, bgu, wd, bd, wt = prm
        gu = h @ wgu + bgu
        gate = jnp.minimum(gu[:, :D_FF], SWIGLU_LIMIT)
        up = jnp.clip(gu[:, D_FF:], -SWIGLU_LIMIT, SWIGLU_LIMIT)
        act = gate * jax.nn.sigmoid(SWIGLU_ALPHA * gate) * (up + 1)
        return acc + wt[:, None].astype(h.dtype) * (act @ wd + bd), None

    out, _ = lax.scan(expert, jnp.zeros_like(h), (w_gu, b_gu, w_d, b_d, comb.T))
    return out


def setup_inputs(seed: int = 0) -> dict:
    key = jax.random.key(seed)
    ks = jax.random.split(key, 32)
    nrm = lambda k, shape, s: jax.random.normal(k, shape, F32) * s
    D = D_MODEL
    rec_b_in = nrm(ks[9], (N_EVEN, REC_P), 0.02)
    f_fwd = sum(REC_COLS[:10])
    f_bwd = sum(REC_COLS[:12])
    rec_b_in = rec_b_in.at[:, f_fwd:f_fwd + ML_HEADS].add(ML_FGATE_BIAS)
    rec_b_in = rec_b_in.at[:, f_bwd:f_bwd + ML_HEADS].add(ML_FGATE_BIAS)
    return {
        'x': nrm(ks[0], (BATCH, SEQ, D), 1.0),
        'c': nrm(ks[1], (BATCH, D), 1.0),
        'ctx': nrm(ks[2], (BATCH, CTX_LEN, D), 1.0),
        'c_ctx': nrm(ks[3], (D,), 1.0),
        'ada_w': nrm(ks[4], (DEPTH, D, 6 * D), 0.3 * D ** -0.5),
        'ada_b': nrm(ks[5], (DEPTH, 6 * D), 0.02),
        'norm_mix_g': 1.0 + nrm(ks[6], (DEPTH, D), 0.02),
        'norm_ffn_g': 1.0 + nrm(ks[7], (DEPTH, D), 0.02),
        'rec_w_in': nrm(ks[8], (N_EVEN, D, REC_P), D ** -0.5),
        'rec_b_in': rec_b_in,
        'rec_w_out': nrm(ks[10], (N_EVEN, HG_W + ML_W, D), (HG_W + ML_W) ** -0.5),
        'hgrn_lb': nrm(ks[11], (DEPTH, HG_HEADS * HG_DK), 0.5),
        'hgrn_out_g': 1.0 + nrm(ks[12], (N_EVEN, HG_W), 0.02),
        'mlstm_out_g': 1.0 + nrm(ks[13], (N_EVEN, ML_W), 0.02),
        'na_w_qkv': nrm(ks[14], (N_ODD, D, 3 * NA_W), D ** -0.5),
        'na_w_out': nrm(ks[15], (N_ODD, NA_W, D), NA_W ** -0.5),
        'na_q_g': 1.0 + nrm(ks[16], (N_ODD, NA_DH), 0.02),
        'na_k_g': 1.0 + nrm(ks[17], (N_ODD, NA_DH), 0.02),
        'na_rpb': nrm(ks[18], (N_ODD, NA_HEADS, 2 * NA_KR - 1, 2 * NA_KC - 1), 0.1),
        'moe_w_router': nrm(ks[19], (DEPTH, D, N_EXPERTS), D ** -0.5),
        'moe_b_router': nrm(ks[20], (DEPTH, N_EXPERTS), 0.01),
        'moe_w_gu': nrm(ks[21], (DEPTH, N_EXPERTS, D, 2 * D_FF), D ** -0.5),
        'moe_b_gu': nrm(ks[22], (DEPTH, N_EXPERTS, 2 * D_FF), 0.02),
        'moe_w_down': nrm(ks[23], (DEPTH, N_EXPERTS, D_FF, D), D_FF ** -0.5),
        'moe_b_down': nrm(ks[24], (DEPTH, N_EXPERTS, D), 0.02),
    }


def reference(x, c, ctx, c_ctx, ada_w, ada_b, norm_mix_g, norm_ffn_g, rec_w_in, rec_b_in, rec_w_out,
              hgrn_lb, hgrn_out_g, mlstm_out_g, na_w_qkv, na_w_out, na_q_g, na_k_g, na_rpb,
              moe_w_router, moe_b_router, moe_w_gu, moe_b_gu, moe_w_down, moe_b_down):
    bsz, t_lat, d = x.shape
    t_ctx = ctx.shape[1]
    s_c = jax.nn.silu(c)
    s_cc = jax.nn.silu(c_ctx)
    lb_w = jax.nn.softmax(hgrn_lb.astype(F32), axis=0)
    lb_all = jnp.maximum(jnp.cumsum(lb_w, axis=0) - lb_w[0], 0.0)
    xc = ctx
    for l in range(DEPTH):
        last = l == DEPTH - 1
        j = l // 2
        mod = s_c @ ada_w[l] + ada_b[l]
        modc = s_cc @ ada_w[l] + ada_b[l]
        sh1, sc1, g1, sh2, sc2, g2 = jnp.split(mod[:, None, :], 6, axis=-1)
        csh1, csc1, cg1, csh2, csc2, cg2 = jnp.split(modc, 6, axis=-1)
        h = rmsnorm(x, norm_mix_g[l]) * (1 + sc1) + sh1
        hc = rmsnorm(xc, norm_mix_g[l]) * (1 + csc1) + csh1
        if l % 2 == 0:
            lb = lb_all[l].reshape(HG_HEADS, 1, HG_DK)
            y, yc = rec_mixer(h, hc, rec_w_in[j], rec_b_in[j], rec_w_out[j], lb, hgrn_out_g[j], mlstm_out_g[j])
        else:
            y, yc = na_mixer(h, hc, na_w_qkv[j], na_w_out[j], na_q_g[j], na_k_g[j], na_rpb[j], not last)
        x = x + g1 * y
        h2 = rmsnorm(x, norm_ffn_g[l]) * (1 + sc2) + sh2
        if last:
            f = moe(h2.reshape(-1, d), moe_w_router[l], moe_b_router[l], moe_w_gu[l], moe_b_gu[l],
                    moe_w_down[l], moe_b_down[l]).reshape(bsz, t_lat, d)
            x = x + g2 * f
        else:
            xc = xc + cg1 * yc
            h2c = rmsnorm(xc, norm_ffn_g[l]) * (1 + csc2) + csh2
            tok = jnp.concatenate([h2, h2c], axis=1).reshape(-1, d)
            f = moe(tok, moe_w_router[l], moe_b_router[l], moe_w_gu[l], moe_b_gu[l],
                    moe_w_down[l], moe_b_down[l]).reshape(bsz, t_lat + t_ctx, d)
            x = x + g2 * f[:, :t_lat]
            xc = xc + cg2 * f[:, t_lat:]
    return x
```

```python
import contextlib
import types
import numpy as np
import concourse.bass as bass
import concourse.mybir as mybir
from concourse.bass_utils import run_bass_kernel_spmd

F32 = mybir.dt.float32
BF16 = mybir.dt.bfloat16
AF = mybir.ActivationFunctionType
ALU = mybir.AluOpType
AX = mybir.AxisListType

D = 1024
KD = 8
NEGB = -30000.0
C_HQ, C_HI, C_MQ, C_MK, C_MV, C_HFF, C_HFB, C_HG, C_MO, C_GT = 0, 512, 1024, 1536, 2048, 2560, 3072, 3584, 4096, 4608
NCW = 4624


def freeze(fn):
    if fn.__closure__ is None:
        return fn
    cells = []
    for c in fn.__closure__:
        try:
            cells.append(types.CellType(c.cell_contents))
        except ValueError:
            cells.append(c)
    g = types.FunctionType(fn.__code__, fn.__globals__, fn.__name__, fn.__defaults__, tuple(cells))
    g.__kwdefaults__ = fn.__kwdefaults__
    return g


class Prog:
    def __init__(self, nc, stack):
        self.nc = nc
        self.stack = stack
        self.engs = ["tensor", "vector", "scalar", "gpsimd", "sync"]
        self.q = {e: [] for e in self.engs}
        self.esem = {e: stack.enter_context(nc.semaphore("es_" + e)) for e in self.engs}
        self.ecnt = {e: 0 for e in self.engs}
        self.waited = {e: {} for e in self.engs}
        self.lastw = {}
        self.readers = {}
        self.nslots = 6
        self.dslots = {}
        for e in ("sync", "gpsimd", "scalar"):
            self.dslots[e] = [[stack.enter_context(nc.semaphore("ds_%s_%d" % (e, i))), 0] for i in range(self.nslots)]
        self.dnext = {e: 0 for e in self.dslots}
        self.ccs = []
        self.nops = 0

    def _wait(self, E, ev):
        sem, val, src = ev
        if src == E and E == "tensor":
            return
        key = id(sem)
        if self.waited[E].get(key, 0) >= val:
            return
        self.waited[E][key] = val
        self.q[E].append(("w", sem, val))

    def _deps(self, E, reads, writes):
        for r in reads:
            ev = self.lastw.get(r)
            if ev is not None:
                self._wait(E, ev)
        for w in writes:
            ev = self.lastw.get(w)
            if ev is not None:
                self._wait(E, ev)
            for ev in self.readers.get(w, {}).values():
                self._wait(E, ev)

    def _commit(self, ev, reads, writes):
        for r in reads:
            self.readers.setdefault(r, {})[id(ev[0])] = ev
        for w in writes:
            self.lastw[w] = ev
            self.readers[w] = {}

    def op(self, E, fn, reads=(), writes=()):
        self._deps(E, reads, writes)
        self.ecnt[E] += 1
        ev = (self.esem[E], self.ecnt[E], E)
        self.q[E].append(("o", freeze(fn), self.esem[E], 1))
        self._commit(ev, reads, writes)
        self.nops += 1

    def dma(self, Q, out, in_, reads=(), writes=()):
        self._deps(Q, reads, writes)
        i = self.dnext[Q]
        self.dnext[Q] = (i + 1) % self.nslots
        slot = self.dslots[Q][i]
        if slot[1] > 0:
            self._wait(Q, (slot[0], slot[1], "dma"))
        slot[1] += 16
        ev = (slot[0], slot[1], "dma")
        fn = lambda e, o=out, a=in_: e.dma_start(out=o, in_=a)
        self.q[Q].append(("o", fn, slot[0], 16))
        self._commit(ev, reads, writes)
        self.nops += 1

    def cc(self, fn, reads=(), writes=()):
        self._deps("gpsimd", reads, writes)
        sem = self.stack.enter_context(self.nc.semaphore("cc_%d" % len(self.ccs)))
        self.ccs.append(sem)
        self.q["gpsimd"].append(("c", freeze(fn), sem))
        self._commit((sem, 1, "cc"), reads, writes)

    def barrier(self):
        evs = [(self.esem[e], self.ecnt[e], e) for e in self.engs if self.ecnt[e] > 0]
        for Q in self.dslots:
            for slot in self.dslots[Q]:
                if slot[1] > 0:
                    evs.append((slot[0], slot[1], "dma"))
        for sem in self.ccs:
            evs.append((sem, 1, "cc"))
        for E in self.engs:
            for ev in evs:
                self._wait(E, ev)

    def flush(self):
        nc = self.nc
        qs = self.q
        with nc.Block() as block:
            def mk(E):
                def body(eng):
                    for it in qs[E]:
                        if it[0] == "w":
                            eng.wait_ge(it[1], it[2])
                        elif it[0] == "c":
                            it[1](eng).then_inc(it[2])
                        else:
                            it[1](eng).then_inc(it[2], it[3])
                return body
            block.tensor(mk("tensor"))
            block.vector(mk("vector"))
            block.scalar(mk("scalar"))
            block.gpsimd(mk("gpsimd"))
            block.sync(mk("sync"))
        self.q = {e: [] for e in self.engs}


def groups_of(cfg):
    gs = [(0, cfg["CTX"])]
    t = cfg["CTX"]
    while t < cfg["CTX"] + cfg["SEQ"]:
        gs.append((t, 512))
        t += 512
    return gs


def na_patterns(rows, nlc):
    pats, rowinfo = {}, []
    for r in range(rows):
        rs = min(max(r - 4, 0), rows - 8)
        c0 = min(rs // 2, nlc - 5)
        key = (rs - r + 7, rs - 2 * c0)
        if key not in pats:
            pats[key] = len(pats)
        rowinfo.append((c0, pats[key]))
    plist = [None] * len(pats)
    for k, v in pats.items():
        plist[v] = k
    return rowinfo, plist


def build(cfg):
    SEQ, CTX, DEPTH, NE, DFF = cfg["SEQ"], cfg["CTX"], cfg["DEPTH"], cfg["NE"], cfg["DFF"]
    T = SEQ + CTX
    EPC = NE // 8
    KF = DFF // 128
    NEVEN, NODD = (DEPTH + 1) // 2, DEPTH // 2
    ROWS = SEQ // 64
    NLC = SEQ // 128
    NCC = CTX // 128
    NCH = T // 64
    GS = groups_of(cfg)
    rowinfo, plist = na_patterns(ROWS, NLC)
    NPAT = len(plist)
    mixers = cfg.get("mixers", "rn")
    nc = bass.Bass("TRN2", target_bir_lowering=False)
    stack = contextlib.ExitStack()
    P = Prog(nc, stack)

    def din(name, shape, dt=F32):
        return nc.dram_tensor(name, list(shape), dt, kind="ExternalInput").ap()

    def dscr(name, shape, dt=F32):
        return nc.dram_tensor(name, list(shape), dt).ap()

    uid = [0]

    def sb(name, shape, dt=F32, st=None):
        uid[0] += 1
        return (st or stack).enter_context(nc.sbuf_tensor("s%d_%s" % (uid[0], name), list(shape), dt))

    def ps(name, shape, dt=F32):
        return stack.enter_context(nc.psum_tensor(name, list(shape), dt))

    def Vv(fn, r=(), w=()):
        P.op("vector", fn, reads=r, writes=w)

    def Aa(fn, r=(), w=()):
        P.op("scalar", fn, reads=r, writes=w)

    def Gg(fn, r=(), w=()):
        P.op("gpsimd", fn, reads=r, writes=w)

    def Tt(fn, r=(), w=()):
        P.op("tensor", fn, reads=r, writes=w)

    xT_in = din("xT", [D, T])
    cT_in = din("cT", [D, 5])
    adaw_in = din("adaw", [DEPTH, D, 768])
    adab_in = din("adab", [DEPTH, 768])
    gmix_in = din("gmix", [DEPTH, D])
    gffn_in = din("gffn", [DEPTH, D])
    m8_in = din("m8", [5, 8])
    sel_in = din("sel", [5, 2])
    mw4_in = din("mw4", [128, 4])
    mr4_in = din("mr4", [128, 4])
    selE_in = din("selE", [NE, EPC, 128])
    ident_in = din("ident", [128, 128])
    wr_in = din("wr", [DEPTH, D, NE])
    br_in = din("br", [DEPTH, NE])
    wgu_in = din("wgu", [DEPTH, EPC, D, 2 * DFF])
    bgu_in = din("bgu", [DEPTH, EPC, 2 * DFF])
    wd_in = din("wd", [DEPTH, EPC, DFF, D])
    bd_in = din("bd", [DEPTH, EPC, D])
    w2_in = din("w2", [NEVEN, D, NCW])
    b2_in = din("b2", [NEVEN, NCW])
    wro_in = din("wro", [NEVEN, D, D])
    lbr_in = din("lbr", [DEPTH, 512])
    ghg_in = din("ghg", [NEVEN, 512])
    gml_in = din("gml", [NEVEN, 512])
    ropec_in = din("ropec", [T, 64])
    ropes_in = din("ropes", [T, 64])
    scanc_in = din("scanc", [64, 580])
    wqkv_in = din("wqkv", [max(NODD, 1), D, 3 * D])
    wno_in = din("wno", [max(NODD, 1), D, D])
    qg_in = din("qg", [max(NODD, 1), 128])
    kg_in = din("kg", [max(NODD, 1), 128])
    br_rel_in = din("brel", [max(NODD, 1), 16, 16, 64, 64])
    bones_in = din("bones", [128, 128])
    out_ext = nc.dram_tensor("out", [D, SEQ], F32, kind="ExternalOutput").ap()

    XT = dscr("XT", [D, T])
    MODIN = dscr("MODIN", [8 * DEPTH * 5, 768])
    MODF = dscr("MODF", [8 * DEPTH * 5, 768])
    GIN = dscr("GIN", [4 * D, T])
    GT = dscr("GT", [4 * D, T])
    FT = dscr("FT", [4 * D, T])
    FR = dscr("FR", [4 * D, T])
    CBIN = dscr("CBIN", [NE, 4 * T])
    CBF = dscr("CBF", [NE, 4 * T])
    OF = dscr("OF", [T, D])
    QT = dscr("QT", [D, T], BF16)
    KT = dscr("KT", [D, T], BF16)
    VA = dscr("VA", [T, 16 * 66], BF16)
    ON = dscr("ON", [T, D], BF16)

    ident_f = sb("ident_f", [128, 128])
    ident_b = sb("ident_b", [128, 128], BF16)
    ones_b = sb("ones_b", [128, 128], BF16)
    mw4 = sb("mw4s", [128, 4])
    mr4 = sb("mr4s", [128, 4])
    eps_t = sb("eps_t", [128, 1])
    MV = sb("MV", [128, DEPTH, 48, 2])
    AV = sb("AV", [128, DEPTH, 2, KD, 2])
    P.dma("sync", ident_f[:], ident_in[:, :], writes=["ident_f"])
    Vv(lambda e: e.tensor_copy(out=ident_b[:], in_=ident_f[:]), ["ident_f"], ["ident_b"])
    Vv(lambda e: e.memset(ones_b[:], 1.0), [], ["ones_b"])
    Vv(lambda e: e.memset(eps_t[:], 1e-6), [], ["eps"])
    P.dma("sync", mw4[:], mw4_in[:, :], writes=["mw4"])
    P.dma("sync", mr4[:], mr4_in[:, :], writes=["mr4"])

    PS = [ps("ps%d" % i, [128, 512]) for i in range(7)]
    PT = ps("pt", [128, 512], BF16)

    def allreduce(src, dst, rkeys, wkeys):
        P.cc(lambda e: e.collective_compute("AllReduce", ALU.add, replica_groups=[list(range(8))],
                                            ins=[src], outs=[dst]), reads=rkeys, writes=wkeys)

    with contextlib.ExitStack() as st:
        xtmp = sb("xtmp", [128, KD, 512], st=st)
        for gi, (t0, n) in enumerate(GS):
            P.dma("sync", xtmp[:, :, :n], xT_in[:, t0:t0 + n].rearrange("(k p) t -> p k t", p=128), writes=["xtmp"])
            P.dma("gpsimd", XT[:, t0:t0 + n].rearrange("(k p) t -> p k t", p=128), xtmp[:, :, :n],
                  reads=["xtmp"], writes=[("XT", gi)])
        cT = sb("cT", [128, KD, 5], st=st)
        sT = sb("sT", [128, KD, 5], st=st)
        P.dma("sync", cT[:], cT_in.rearrange("(k p) r -> p k r", p=128), writes=["cT"])
        Aa(lambda e: e.activation(out=sT[:], in_=cT[:], func=AF.Silu), ["cT"], ["sT"])
        adaw = sb("adaw_s", [128, KD, 768], st=st)
        adab = sb("adab_s", [5, 768], st=st)
        modp = sb("modp", [5, 768], st=st)
        modm = sb("modm", [5, 8, 768], st=st)
        m8 = sb("m8s", [5, 8], st=st)
        P.dma("sync", m8[:], m8_in[:, :], writes=["m8"])
        for l in range(DEPTH):
            P.dma("sync", adaw[:], adaw_in[l].rearrange("(k p) c -> p k c", p=128), writes=["adaw"])
            P.dma("sync", adab[:], adab_in[l:l + 1, :].partition_broadcast(5), writes=["adab"])
            for hf in range(2):
                for k in range(KD):
                    Tt(lambda e, k=k, hf=hf: e.matmul(PS[hf][:5, :384], lhsT=sT[:, k, :], rhs=adaw[:, k, hf * 384:(hf + 1) * 384],
                                                      start=(k == 0), stop=(k == KD - 1)), ["sT", "adaw"], [("ps", hf)])
                Vv(lambda e, hf=hf: e.tensor_tensor(out=modp[:, hf * 384:(hf + 1) * 384], in0=PS[hf][:5, :384],
                                                    in1=adab[:, hf * 384:(hf + 1) * 384], op=ALU.add), [("ps", hf), "adab"], ["modp"])
            for j in range(8):
                Vv(lambda e, j=j: e.tensor_scalar(out=modm[:, j, :], in0=modp[:], scalar1=m8[:, j:j + 1], scalar2=None, op0=ALU.mult),
                   ["modp", "m8"], ["modm"])
            P.dma("gpsimd", MODIN.rearrange("(j l r) c -> l r j c", j=8, l=DEPTH)[l], modm[:], reads=["modm"], writes=["MODIN"])
        allreduce(MODIN[:, :], MODF[:, :], ["MODIN"], ["MODF"])
        modrows = sb("modrows", [5, 8, 768], st=st)
        selc = sb("selc", [5, 2], st=st)
        gmix = sb("gmix_s", [128, DEPTH, KD], st=st)
        gffn = sb("gffn_s", [128, DEPTH, KD], st=st)
        P.dma("sync", selc[:], sel_in[:, :], writes=["selc"])
        for l in range(DEPTH):
            P.dma("sync", modrows[:], MODF.rearrange("(j l r) c -> l r j c", j=8, l=DEPTH)[l], reads=["MODF"], writes=["modrows"])
            for q in range(48):
                j, cc = (q * 128) // 768, (q * 128) % 768
                Tt(lambda e, j=j, cc=cc, q=q: e.matmul(PS[2][:, q * 2:q * 2 + 2], lhsT=modrows[:, j, cc:cc + 128], rhs=selc[:], start=True, stop=True),
                   ["modrows", "selc"], [("ps", 2)])
            Vv(lambda e, l=l: e.tensor_copy(out=MV[:, l, :, :], in_=PS[2][:, :96].rearrange("p (q c) -> p q c", c=2)), [("ps", 2)], ["MV"])
        for l in range(DEPTH):
            for k in range(KD):
                P.dma("sync", gmix[:, l, k:k + 1], gmix_in[l, k * 128:(k + 1) * 128].rearrange("(p o) -> p o", o=1), writes=["gmix"])
                P.dma("sync", gffn[:, l, k:k + 1], gffn_in[l, k * 128:(k + 1) * 128].rearrange("(p o) -> p o", o=1), writes=["gffn"])
        for l in range(DEPTH):
            for w, (g, q0) in enumerate(((gmix, 8), (gffn, 32))):
                Vv(lambda e, l=l, w=w, q0=q0: e.tensor_scalar(out=AV[:, l, w], in0=MV[:, l, q0:q0 + 8, :], scalar1=1.0, scalar2=None, op0=ALU.add),
                   ["MV"], ["AV"])
                Vv(lambda e, l=l, w=w, g=g: e.tensor_tensor(out=AV[:, l, w], in0=AV[:, l, w], in1=g[:, l, :].unsqueeze(2).to_broadcast([128, KD, 2]), op=ALU.mult),
                   ["AV", "gmix", "gffn"], ["AV"])
        P.barrier()
        P.flush()

    def mn_tiles(st, nmax, nbuf=2):
        return dict(
            xg=[sb("xg%d" % i, [128, KD, nmax], st=st) for i in range(nbuf)],
            sq=sb("sq", [128, KD, nmax], BF16, st=st),
            rstd=sb("rstd", [128, nmax], st=st),
            xn=sb("xn", [128, nmax], st=st),
            hT=[sb("hT%d" % i, [128, KD, nmax], BF16, st=st) for i in range(nbuf)],
        )

    def modnorm(tl, l, which, xkey, t0, n, buf, out_f32=None):
        isc = 0 if t0 >= CTX else 1
        x_ = tl["xg"][buf]
        h_ = tl["hT"][buf] if out_f32 is None else out_f32
        sq, rstd, xn = tl["sq"], tl["rstd"], tl["xn"]
        xk, hk = ("xg", buf), ("hT", buf)
        P.dma("sync", x_[:, :, :n], XT[:, t0:t0 + n].rearrange("(k p) t -> p k t", p=128), reads=[xkey], writes=[xk])
        Aa(lambda e: e.activation(out=sq[:, :, :n], in_=x_[:, :, :n], func=AF.Square), [xk], ["sq"])
        for k in range(KD):
            Tt(lambda e, k=k: e.matmul(PS[0][:, :n], lhsT=ones_b[:], rhs=sq[:, k, :n], start=(k == 0), stop=(k == KD - 1)),
               ["sq", "ones_b"], [("ps", 0)])
        Aa(lambda e: e.activation(out=rstd[:, :n], in_=PS[0][:, :n], func=AF.Sqrt, scale=1.0 / D, bias=eps_t[:, 0:1]),
           [("ps", 0), "eps"], ["rstd"])
        Vv(lambda e: e.reciprocal(out=rstd[:, :n], in_=rstd[:, :n]), ["rstd"], ["rstd"])
        bq = 0 if which == 0 else 24
        for k in range(KD):
            Vv(lambda e, k=k: e.tensor_tensor(out=xn[:, :n], in0=x_[:, k, :n], in1=rstd[:, :n], op=ALU.mult), [xk, "rstd"], ["xn"])
            Aa(lambda e, k=k: e.activation(out=h_[:, k, :n], in_=xn[:, :n], func=AF.Identity,
                                           scale=AV[:, l, which, k, isc:isc + 1], bias=MV[:, l, bq + k, isc:isc + 1]),
               ["xn", "AV", "MV"], [hk])
        return hk

    def load_cast(st_tile, dst, src_rows, ncols, key_st, key_dst):
        P.dma("sync", st_tile[:, :ncols], src_rows, writes=[key_st])
        Vv(lambda e: e.tensor_copy(out=dst, in_=st_tile[:, :ncols]), [key_st], [key_dst])

    def rec_layer(l):
        j = l // 2
        with contextlib.ExitStack() as st:
            P.barrier()
            win_b = sb("win_b", [128, KD, NCW], BF16, st=st)
            wout_b = sb("wout_b", [128, KD, D], BF16, st=st)
            bhi = sb("bhi", [1, NCW], BF16, st=st)
            blo = sb("blo", [1, NCW], BF16, st=st)
            scc = sb("scc", [64, 580], st=st)
            lb_bc = sb("lb_bc", [64, 512], st=st)
            oml_bc = sb("oml_bc", [64, 512], st=st)
            ghg_bc = sb("ghg_bc", [64, 512], st=st)
            gml_bc = sb("gml_bc", [64, 512], st=st)
            st2 = contextlib.ExitStack()
            wst = sb("wst", [128, NCW], st=st2)
            for k in range(KD):
                load_cast(wst, win_b[:, k, :], w2_in[j, k * 128:(k + 1) * 128, :], NCW, "wst", "win_b")
            for k in range(KD):
                load_cast(wst, wout_b[:, k, :], wro_in[j, k * 128:(k + 1) * 128, :], D, "wst", "wout_b")
            b2f = sb("b2f", [1, NCW], st=st2)
            bt = sb("bt", [1, NCW], st=st2)
            P.dma("sync", b2f[:], b2_in[j:j + 1, :], writes=["b2f"])
            Vv(lambda e: e.tensor_copy(out=bhi[:], in_=b2f[:]), ["b2f"], ["bhi"])
            Vv(lambda e: e.tensor_copy(out=bt[:], in_=bhi[:]), ["bhi"], ["bt"])
            Vv(lambda e: e.tensor_tensor(out=bt[:], in0=b2f[:], in1=bt[:], op=ALU.subtract), ["b2f", "bt"], ["bt"])
            Vv(lambda e: e.tensor_copy(out=blo[:], in_=bt[:]), ["bt"], ["blo"])
            P.dma("sync", scc[:], scanc_in[:, :], writes=["scc"])
            Mcum = [scc[:, 0:64], scc[:, 64:128]]
            mask = [scc[:, 128:192], scc[:, 192:256]]
            tri = [scc[:, 256:320], scc[:, 320:384]]
            ones64 = scc[:, 384:448]
            selm = [scc[:, 448:450], scc[:, 450:452]]
            lbr = sb("lbr", [64, DEPTH, 512], st=st2)
            lbm = sb("lbm", [64, 512], st=st2)
            lbs = sb("lbs", [64, 512], st=st2)
            for i in range(DEPTH):
                P.dma("sync", lbr[:, i, :], lbr_in[i:i + 1, :].partition_broadcast(64), writes=["lbr"])
            Vv(lambda e: e.tensor_copy(out=lbm[:], in_=lbr[:, 0, :]), ["lbr"], ["lbm"])
            for i in range(1, DEPTH):
                Vv(lambda e, i=i: e.tensor_tensor(out=lbm[:], in0=lbm[:], in1=lbr[:, i, :], op=ALU.max), ["lbr", "lbm"], ["lbm"])
            for i in range(DEPTH):
                Vv(lambda e, i=i: e.tensor_tensor(out=lbr[:, i, :], in0=lbr[:, i, :], in1=lbm[:], op=ALU.subtract), ["lbr", "lbm"], ["lbr"])
            Aa(lambda e: e.activation(out=lbr[:], in_=lbr[:], func=AF.Exp), ["lbr"], ["lbr"])
            Vv(lambda e: e.tensor_copy(out=lbs[:], in_=lbr[:, 0, :]), ["lbr"], ["lbs"])
            for i in range(1, DEPTH):
                Vv(lambda e, i=i: e.tensor_tensor(out=lbs[:], in0=lbs[:], in1=lbr[:, i, :], op=ALU.add), ["lbr", "lbs"], ["lbs"])
            Vv(lambda e: e.reciprocal(out=lbs[:], in_=lbs[:]), ["lbs"], ["lbs"])
            Vv(lambda e: e.memset(lb_bc[:], 0.0), [], ["lb_bc"])
            for i in range(1, l + 1):
                Vv(lambda e, i=i: e.tensor_tensor(out=lb_bc[:], in0=lb_bc[:], in1=lbr[:, i, :], op=ALU.add), ["lbr", "lb_bc"], ["lb_bc"])
            Vv(lambda e: e.tensor_tensor(out=lb_bc[:], in0=lb_bc[:], in1=lbs[:], op=ALU.mult), ["lbs", "lb_bc"], ["lb_bc"])
            Vv(lambda e: e.tensor_scalar(out=lb_bc[:], in0=lb_bc[:], scalar1=0.0, scalar2=None, op0=ALU.max), ["lb_bc"], ["lb_bc"])
            Vv(lambda e: e.tensor_scalar(out=oml_bc[:], in0=lb_bc[:], scalar1=-1.0, scalar2=1.0, op0=ALU.mult, op1=ALU.add), ["lb_bc"], ["oml_bc"])
            P.dma("sync", ghg_bc[:], ghg_in[j:j + 1, :].partition_broadcast(64), writes=["ghg_bc"])
            P.dma("sync", gml_bc[:], gml_in[j:j + 1, :].partition_broadcast(64), writes=["gml_bc"])
            P.barrier()
            P.flush()
            st2.close()
            tl = mn_tiles(st, 64, 1)
            qs = sb("qs", [64, 512], st=st)
            v_b = sb("v_b", [64, 512], BF16, st=st)
            sg = sb("sg", [64, 512], st=st)
            la = sb("la", [64, 512], st=st)
            kk = sb("kk", [64, 512], st=st)
            eb = sb("eb", [64, 512], st=st)
            enb = sb("enb", [64, 512], st=st)
            qt = sb("qt", [64, 512], BF16, st=st)
            kt = sb("kt", [64, 512], BF16, st=st)
            em = sb("em", [128, 4, 2], st=st)
            qkT = sb("qkT", [128, 8, 64], BF16, st=st)
            S = sb("S", [128, 4, 128], st=st)
            Sm = sb("Sm", [128, 4, 128], st=st)
            Sm_b = sb("Sm_b", [128, 4, 128], BF16, st=st)
            att_b = sb("att_b", [64, 4, 64], BF16, st=st)
            rc = sb("rc", [64, 64], st=st)
            rs_ = sb("rs_", [64, 64], st=st)
            t1 = sb("t1", [64, 4, 64], st=st)
            t2 = sb("t2", [64, 4, 64], st=st)
            rq = sb("rq", [64, 4, 64], st=st)
            rk = sb("rk", [64, 4, 64], st=st)
            vaug = sb("vaug", [64, 4, 129], BF16, st=st)
            sgm = sb("sgm", [64, 4], st=st)
            lf = sb("lf", [64, 4], st=st)
            gi_ = sb("gi_", [64, 4], st=st)
            u_ = sb("u_", [64, 4], st=st)
            g_ = sb("g_", [64, 4], st=st)
            ebend = sb("ebend", [64, 4], st=st)
            mqt = sb("mqt", [64, 4, 64], BF16, st=st)
            mkt = sb("mkt", [64, 4, 64], BF16, st=st)
            mqkT = sb("mqkT", [64, 8, 64], BF16, st=st)
            matt_b = sb("matt_b", [64, 4, 64], BF16, st=st)
            Cs = sb("Cs", [64, 4, 129], st=st)
            C_b = sb("C_b", [64, 4, 129], BF16, st=st)
            dn = sb("dn", [64, 4], st=st)
            hm = sb("hm", [64, 4, 128], st=st)
            ofs = sb("ofs", [64, D], st=st)
            og = sb("og", [64, 4, 128], st=st)
            sq2 = sb("sq2", [64, 4, 128], st=st)
            ss = sb("ss", [64, 4], st=st)
            sgh = sb("sgh", [64, 512], st=st)
            Y = sb("Y", [64, D], BF16, st=st)
            yT = sb("yT", [128, 8, 64], BF16, st=st)
            ones1 = sb("ones1", [1, 64], BF16, st=st)
            Vv(lambda e: e.memset(ones1[:], 1.0), [], ["ones1"])
            Vv(lambda e: e.memset(vaug[:], 1.0), [], ["vaug"])
            hT = tl["hT"][0]
            xg = tl["xg"][0]

            def proj(c0, ncol, bank):
                pk = ("ps", bank)
                for k in range(KD):
                    Tt(lambda e, k=k: e.matmul(PS[bank][:64, :ncol], lhsT=hT[:, k, :64], rhs=win_b[:, k, c0:c0 + ncol], start=(k == 0), stop=False),
                       [("hT", 0), "win_b"], [pk])
                Tt(lambda e: e.matmul(PS[bank][:64, :ncol], lhsT=ones1[:, :], rhs=bhi[:, c0:c0 + ncol], start=False, stop=False), ["ones1", "bhi"], [pk])
                Tt(lambda e: e.matmul(PS[bank][:64, :ncol], lhsT=ones1[:, :], rhs=blo[:, c0:c0 + ncol], start=False, stop=True), ["ones1", "blo"], [pk])
                return pk

            def bc4(a, w):
                return a.unsqueeze(2).to_broadcast([64, 4, w])

            def mbc(a):
                return a.unsqueeze(1).to_broadcast([64, 4, 64])

            def v3(a, w):
                return a.rearrange("p (h w) -> p h w", w=w)

            def headnorm(src, gbc, gkey):
                Vv(lambda e: e.tensor_tensor(out=sq2[:], in0=src[:], in1=src[:], op=ALU.mult), [src_key[id(src)]], ["sq2"])
                Vv(lambda e: e.tensor_reduce(out=ss[:], in_=sq2[:], axis=AX.X, op=ALU.add), ["sq2"], ["ss"])
                Vv(lambda e: e.tensor_scalar(out=ss[:], in0=ss[:], scalar1=1.0 / 128, scalar2=1e-6, op0=ALU.mult, op1=ALU.add), ["ss"], ["ss"])
                Aa(lambda e: e.activation(out=ss[:], in_=ss[:], func=AF.Sqrt), ["ss"], ["ss"])
                Vv(lambda e: e.reciprocal(out=ss[:], in_=ss[:]), ["ss"], ["ss"])
                Vv(lambda e: e.tensor_tensor(out=src[:], in0=src[:], in1=bc4(ss[:], 128), op=ALU.mult), [src_key[id(src)], "ss"], [src_key[id(src)]])
                Gg(lambda e: e.tensor_tensor(out=src[:], in0=src[:], in1=v3(gbc[:], 128), op=ALU.mult), [src_key[id(src)], gkey], [src_key[id(src)]])

            src_key = {id(og): "og", id(hm): "hm"}

            def step(ci, dirn):
                t0 = ci * 64
                isc = 0 if t0 >= CTX else 1
                xkey = ("XT", ci)
                modnorm(tl, l, 0, xkey, t0, 64, 0)
                P.dma("scalar", rc[:], ropec_in[t0:t0 + 64, :], writes=["rc"])
                P.dma("scalar", rs_[:], ropes_in[t0:t0 + 64, :], writes=["rs_"])
                pk = proj(C_HQ, 512, 1)
                Aa(lambda e: e.activation(out=qs[:], in_=PS[1][:64, :], func=AF.Silu), [pk], ["qs"])
                pk = proj(C_HI, 512, 2)
                Aa(lambda e: e.copy(out=v_b[:], in_=PS[2][:64, :]), [pk], ["v_b"])
                pk = proj(C_HFF if dirn == 0 else C_HFB, 512, 1)
                Aa(lambda e: e.activation(out=sg[:], in_=PS[1][:64, :], func=AF.Sigmoid), [pk], ["sg"])
                Vv(lambda e: e.tensor_tensor(out=sg[:], in0=sg[:], in1=oml_bc[:], op=ALU.mult), ["sg", "oml_bc"], ["sg"])
                Vv(lambda e: e.tensor_tensor(out=sg[:], in0=sg[:], in1=lb_bc[:], op=ALU.add), ["sg", "lb_bc"], ["sg"])
                Aa(lambda e: e.activation(out=la[:], in_=sg[:], func=AF.Ln), ["sg"], ["la"])
                Vv(lambda e: e.tensor_scalar(out=kk[:], in0=sg[:], scalar1=-1.0, scalar2=1.0, op0=ALU.mult, op1=ALU.add), ["sg"], ["kk"])
                Tt(lambda e: e.matmul(PS[3][:64, :], lhsT=Mcum[dirn], rhs=la[:], start=True, stop=True), ["la", "scc"], [("ps", 3)])
                Aa(lambda e: e.activation(out=eb[:], in_=PS[3][:64, :], func=AF.Exp), [("ps", 3)], ["eb"])
                Aa(lambda e: e.activation(out=enb[:], in_=PS[3][:64, :], func=AF.Exp, scale=-1.0), [("ps", 3)], ["enb"])
                Vv(lambda e: e.scalar_tensor_tensor(out=qt[:], in0=qs[:], scalar=128.0 ** -0.5, in1=eb[:], op0=ALU.mult, op1=ALU.mult), ["qs", "eb"], ["qt"])
                Vv(lambda e: e.tensor_tensor(out=kt[:], in0=kk[:], in1=enb[:], op=ALU.mult), ["kk", "enb"], ["kt"])
                for h in range(4):
                    Tt(lambda e, h=h: e.matmul(PS[0][:, h * 2:h * 2 + 2], lhsT=la[:, h * 128:(h + 1) * 128], rhs=selm[dirn], start=True, stop=True),
                       ["la", "scc"], [("ps", 0)])
                Aa(lambda e: e.activation(out=em[:], in_=PS[0][:, 0:8].rearrange("p (h c) -> p h c", c=2), func=AF.Exp), [("ps", 0)], ["em"])
                for h in range(4):
                    Tt(lambda e, h=h: e.transpose(PT[:, h * 64:(h + 1) * 64], qt[:, h * 128:(h + 1) * 128], ident_b[:64, :64]), ["qt", "ident_b"], ["PT"])
                    Tt(lambda e, h=h: e.transpose(PT[:, (4 + h) * 64:(5 + h) * 64], kt[:, h * 128:(h + 1) * 128], ident_b[:64, :64]), ["kt", "ident_b"], ["PT"])
                Vv(lambda e: e.tensor_copy(out=qkT[:], in_=PT[:, 0:512].rearrange("p (a t) -> p a t", t=64)), ["PT"], ["qkT"])
                Vv(lambda e: e.tensor_tensor(out=Sm[:], in0=S[:], in1=em[:, :, 0:1].to_broadcast([128, 4, 128]), op=ALU.mult), ["S", "em"], ["Sm"])
                Aa(lambda e: e.copy(out=Sm_b[:], in_=Sm[:]), ["Sm"], ["Sm_b"])
                for h in range(4):
                    Tt(lambda e, h=h: e.matmul(PS[4][:64, h * 64:(h + 1) * 64], lhsT=qkT[:, 4 + h, :], rhs=qkT[:, h, :], start=True, stop=True), ["qkT"], [("ps", 4)])
                Vv(lambda e: e.tensor_tensor(out=att_b[:], in0=v3(PS[4][:64, 0:256], 64), in1=mbc(mask[dirn]), op=ALU.mult), [("ps", 4), "scc"], ["att_b"])
                for h in range(4):
                    Tt(lambda e, h=h: e.matmul(PS[5][:64, h * 128:(h + 1) * 128], lhsT=att_b[:, h, :], rhs=v_b[:, h * 128:(h + 1) * 128], start=True, stop=False),
                       ["att_b", "v_b"], [("ps", 5)])
                    Tt(lambda e, h=h: e.matmul(PS[5][:64, h * 128:(h + 1) * 128], lhsT=qkT[:, h, :], rhs=Sm_b[:, h, :], start=False, stop=True),
                       ["qkT", "Sm_b"], [("ps", 5)])
                for h in range(4):
                    Tt(lambda e, h=h: e.matmul(PS[6][:, h * 128:(h + 1) * 128], lhsT=kt[:, h * 128:(h + 1) * 128], rhs=v_b[:, h * 128:(h + 1) * 128], start=True, stop=True),
                       ["kt", "v_b"], [("ps", 6)])
                Vv(lambda e: e.tensor_tensor(out=S[:], in0=Sm[:], in1=v3(PS[6][:, :], 128), op=ALU.add), ["Sm", ("ps", 6)], ["S"])
                Vv(lambda e: e.tensor_tensor(out=S[:], in0=S[:], in1=em[:, :, 1:2].to_broadcast([128, 4, 128]), op=ALU.mult), ["S", "em"], ["S"])
                if dirn == 0:
                    Vv(lambda e: e.tensor_copy(out=ofs[:, 0:512], in_=PS[5][:64, :]), [("ps", 5)], ["ofs"])
                else:
                    P.dma("scalar", ofs[:], OF[t0:t0 + 64, :], reads=[("OF", ci)], writes=["ofs"])
                    Vv(lambda e: e.tensor_tensor(out=og[:], in0=v3(PS[5][:64, :], 128), in1=v3(ofs[:, 0:512], 128), op=ALU.add), [("ps", 5), "ofs"], ["og"])
                pk = proj(C_MQ, 512, 2)
                Vv(lambda e: e.tensor_tensor(out=t1[:], in0=v3(PS[2][:64, 0:256], 64), in1=mbc(rc[:]), op=ALU.mult), [pk, "rc"], ["t1"])
                Vv(lambda e: e.tensor_tensor(out=t2[:], in0=v3(PS[2][:64, 256:512], 64), in1=mbc(rs_[:]), op=ALU.mult), [pk, "rs_"], ["t2"])
                Gg(lambda e: e.tensor_tensor(out=rq[:], in0=t1[:], in1=t2[:], op=ALU.add), ["t1", "t2"], ["rq"])
                pk = proj(C_MK, 512, 1)
                Vv(lambda e: e.tensor_tensor(out=t1[:], in0=v3(PS[1][:64, 0:256], 64), in1=mbc(rc[:]), op=ALU.mult), [pk, "rc"], ["t1"])
                Vv(lambda e: e.tensor_tensor(out=t2[:], in0=v3(PS[1][:64, 256:512], 64), in1=mbc(rs_[:]), op=ALU.mult), [pk, "rs_"], ["t2"])
                Gg(lambda e: e.tensor_tensor(out=rk[:], in0=t1[:], in1=t2[:], op=ALU.add), ["t1", "t2"], ["rk"])
                pk = proj(C_MV, 512, 2)
                Aa(lambda e: e.copy(out=vaug[:, :, 0:128], in_=v3(PS[2][:64, :], 128)), [pk], ["vaug"])
                pk = proj(C_GT + dirn * 8, 8, 1)
                Aa(lambda e: e.activation(out=sgm[:], in_=PS[1][:64, 4:8], func=AF.Sigmoid), [pk], ["sgm"])
                Aa(lambda e: e.activation(out=lf[:], in_=sgm[:], func=AF.Ln), ["sgm"], ["lf"])
                Vv(lambda e: e.tensor_copy(out=gi_[:], in_=PS[1][:64, 0:4]), [pk], ["gi_"])
                Tt(lambda e: e.matmul(PS[3][:64, 0:4], lhsT=tri[dirn], rhs=lf[:], start=True, stop=True), ["lf", "scc"], [("ps", 3)])
                Tt(lambda e: e.matmul(PS[3][:64, 8:12], lhsT=ones64, rhs=lf[:], start=True, stop=True), ["lf", "scc"], [("ps", 3)])
                Aa(lambda e: e.activation(out=u_[:], in_=PS[3][:64, 0:4], func=AF.Exp), [("ps", 3)], ["u_"])
                Vv(lambda e: e.tensor_tensor(out=gi_[:], in0=gi_[:], in1=PS[3][:64, 0:4], op=ALU.subtract), ["gi_", ("ps", 3)], ["gi_"])
                Aa(lambda e: e.activation(out=g_[:], in_=gi_[:], func=AF.Exp), ["gi_"], ["g_"])
                Aa(lambda e: e.activation(out=ebend[:], in_=PS[3][:64, 8:12], func=AF.Exp), [("ps", 3)], ["ebend"])
                Vv(lambda e: e.tensor_tensor(out=mqt[:], in0=rq[:], in1=bc4(u_[:], 64), op=ALU.mult), ["rq", "u_"], ["mqt"])
                Vv(lambda e: e.scalar_tensor_tensor(out=mkt[:], in0=rk[:], scalar=0.125, in1=bc4(g_[:], 64), op0=ALU.mult, op1=ALU.mult), ["rk", "g_"], ["mkt"])
                for h in range(4):
                    Tt(lambda e, h=h: e.transpose(PT[:64, h * 64:(h + 1) * 64], mqt[:, h, :], ident_b[:64, :64]), ["mqt", "ident_b"], ["PT"])
                    Tt(lambda e, h=h: e.transpose(PT[:64, (4 + h) * 64:(5 + h) * 64], mkt[:, h, :], ident_b[:64, :64]), ["mkt", "ident_b"], ["PT"])
                Vv(lambda e: e.tensor_copy(out=mqkT[:], in_=PT[:64, 0:512].rearrange("p (a t) -> p a t", t=64)), ["PT"], ["mqkT"])
                for h in range(4):
                    Tt(lambda e, h=h: e.matmul(PS[4][:64, 256 + h * 64:256 + (h + 1) * 64], lhsT=mqkT[:, 4 + h, :], rhs=mqkT[:, h, :], start=True, stop=True),
                       ["mqkT"], [("ps", 4)])
                Vv(lambda e: e.tensor_tensor(out=matt_b[:], in0=v3(PS[4][:64, 256:512], 64), in1=mbc(mask[dirn]), op=ALU.mult), [("ps", 4), "scc"], ["matt_b"])
                for h in range(4):
                    bank, col = 5 + h // 2, (h % 2) * 129
                    Tt(lambda e, h=h, bank=bank, col=col: e.matmul(PS[bank][:64, col:col + 129], lhsT=matt_b[:, h, :], rhs=vaug[:, h, :], start=True, stop=False),
                       ["matt_b", "vaug"], [("ps", bank)])
                    Tt(lambda e, h=h, bank=bank, col=col: e.matmul(PS[bank][:64, col:col + 129], lhsT=mqkT[:, h, :], rhs=C_b[:, h, :], start=False, stop=True),
                       ["mqkT", "C_b"], [("ps", bank)])
                for hf in range(2):
                    pv = PS[5 + hf][:64, 0:258].rearrange("p (h w) -> p h w", w=129)
                    Aa(lambda e, hf=hf, pv=pv: e.activation(out=dn[:, 2 * hf:2 * hf + 2].unsqueeze(2), in_=pv[:, :, 128:129], func=AF.Abs),
                       [("ps", 5 + hf)], ["dn"])
                Vv(lambda e: e.tensor_scalar(out=dn[:], in0=dn[:], scalar1=1.0, scalar2=None, op0=ALU.max), ["dn"], ["dn"])
                Vv(lambda e: e.reciprocal(out=dn[:], in_=dn[:]), ["dn"], ["dn"])
                for hf in range(2):
                    pv = PS[5 + hf][:64, 0:258].rearrange("p (h w) -> p h w", w=129)
                    Vv(lambda e, hf=hf, pv=pv: e.tensor_tensor(out=hm[:, 2 * hf:2 * hf + 2, :], in0=pv[:, :, 0:128],
                                                               in1=dn[:, 2 * hf:2 * hf + 2].unsqueeze(2).to_broadcast([64, 2, 128]), op=ALU.mult),
                       [("ps", 5 + hf), "dn"], ["hm"])
                for h in range(4):
                    bank, col = 1 + h // 2, (h % 2) * 129
                    Tt(lambda e, h=h, bank=bank, col=col: e.matmul(PS[bank][:64, col:col + 129], lhsT=mkt[:, h, :], rhs=vaug[:, h, :], start=True, stop=True),
                       ["mkt", "vaug"], [("ps", bank)])
                for hf in range(2):
                    pv = PS[1 + hf][:64, 0:258].rearrange("p (h w) -> p h w", w=129)
                    Vv(lambda e, hf=hf, pv=pv: e.tensor_tensor(out=Cs[:, 2 * hf:2 * hf + 2, :], in0=Cs[:, 2 * hf:2 * hf + 2, :], in1=pv, op=ALU.add),
                       ["Cs", ("ps", 1 + hf)], ["Cs"])
                Vv(lambda e: e.tensor_tensor(out=Cs[:], in0=Cs[:], in1=bc4(ebend[:], 129), op=ALU.mult), ["Cs", "ebend"], ["Cs"])
                Aa(lambda e: e.copy(out=C_b[:], in_=Cs[:]), ["Cs"], ["C_b"])
                if dirn == 0:
                    Gg(lambda e: e.tensor_copy(out=ofs[:, 512:1024], in_=hm[:].rearrange("p h w -> p (h w)")), ["hm"], ["ofs"])
                    P.dma("scalar", OF[t0:t0 + 64, :], ofs[:], reads=["ofs"], writes=[("OF", ci)])
                    return
                Gg(lambda e: e.tensor_tensor(out=hm[:], in0=hm[:], in1=v3(ofs[:, 512:1024], 128), op=ALU.add), ["hm", "ofs"], ["hm"])
                headnorm(og, ghg_bc, "ghg_bc")
                pk = proj(C_HG, 512, 1)
                Aa(lambda e: e.activation(out=sgh[:], in_=PS[1][:64, :], func=AF.Silu), [pk], ["sgh"])
                Vv(lambda e: e.tensor_tensor(out=Y[:, 0:512], in0=og[:].rearrange("p h w -> p (h w)"), in1=sgh[:], op=ALU.mult), ["og", "sgh"], ["Y"])
                headnorm(hm, gml_bc, "gml_bc")
                pk = proj(C_MO, 512, 2)
                Aa(lambda e: e.activation(out=sgh[:], in_=PS[2][:64, :], func=AF.Sigmoid), [pk], ["sgh"])
                Vv(lambda e: e.tensor_tensor(out=Y[:, 512:1024], in0=hm[:].rearrange("p h w -> p (h w)"), in1=sgh[:], op=ALU.mult), ["hm", "sgh"], ["Y"])
                for a in range(8):
                    Tt(lambda e, a=a: e.transpose(PT[:, a * 64:(a + 1) * 64], Y[:, a * 128:(a + 1) * 128], ident_b[:64, :64]), ["Y", "ident_b"], ["PT"])
                Vv(lambda e: e.tensor_copy(out=yT[:], in_=PT[:, 0:512].rearrange("p (a t) -> p a t", t=64)), ["PT"], ["yT"])
                for dc in range(KD):
                    for k in range(KD):
                        Tt(lambda e, dc=dc, k=k: e.matmul(PS[0][:, dc * 64:(dc + 1) * 64], lhsT=wout_b[:, k, dc * 128:(dc + 1) * 128], rhs=yT[:, k, :],
                                                          start=(k == 0), stop=(k == KD - 1)), ["yT", "wout_b"], [("ps", 0)])
                for dc in range(KD):
                    Vv(lambda e, dc=dc: e.scalar_tensor_tensor(out=xg[:, dc, :64], in0=PS[0][:, dc * 64:(dc + 1) * 64], scalar=MV[:, l, 16 + dc, isc:isc + 1],
                                                               in1=xg[:, dc, :64], op0=ALU.mult, op1=ALU.add), [("ps", 0), ("xg", 0), "MV"], [("xg", 0)])
                P.dma("gpsimd", XT[:, t0:t0 + 64].rearrange("(k p) t -> p k t", p=128), xg[:, :, :64], reads=[("xg", 0)], writes=[xkey])

            ncc = CTX // 64
            for dirn in range(2):
                Vv(lambda e: e.memset(S[:], 0.0), [], ["S"])
                Vv(lambda e: e.memset(Cs[:], 0.0), [], ["Cs"])
                Vv(lambda e: e.memset(C_b[:], 0.0), [], ["C_b"])
                if dirn == 0:
                    order = list(range(NCH))
                else:
                    order = list(range(ncc - 1, -1, -1)) + list(range(NCH - 1, ncc - 1, -1))
                for ci in order:
                    step(ci, dirn)
            P.barrier()
            P.flush()

    def na_layer(l):
        j = l // 2
        with contextlib.ExitStack() as st:
            P.barrier()
            tl = mn_tiles(st, 512, 2)
            wst = sb("wst", [128, 3 * D], st=st)
            wq_b = sb("wq_b", [128, KD, 3 * D], BF16, st=st)
            for k in range(KD):
                load_cast(wst, wq_b[:, k, :], wqkv_in[j, k * 128:(k + 1) * 128, :], 3 * D, "wst", "wq_b")
            bones = sb("bones", [128, 128], st=st)
            bones_b = sb("bones_b", [128, 128], BF16, st=st)
            P.dma("sync", bones[:], bones_in[:, :], writes=["bones"])
            Vv(lambda e: e.tensor_copy(out=bones_b[:], in_=bones[:]), ["bones"], ["bones_b"])
            gq = sb("gq", [128, 2], st=st)
            P.dma("sync", gq[:, 0:1], qg_in[j, :].rearrange("(p o) -> p o", o=1), writes=["gq"])
            P.dma("sync", gq[:, 1:2], kg_in[j, :].rearrange("(p o) -> p o", o=1), writes=["gq"])
            sqq = sb("sqq", [128, 512], BF16, st=st)
            rsq = sb("rsq", [128, 512], st=st)
            qn = [sb("qn%d" % i, [128, 512], BF16, st=st) for i in range(2)]
            va = [sb("va%d" % i, [128, 16, 66], BF16, st=st) for i in range(2)]
            for i in range(2):
                Vv(lambda e, i=i: e.memset(va[i][:], 1.0), [], [("va", i)])
            cnt = 0
            for gi, (t0, n) in enumerate(GS):
                buf = gi % 2
                hk = modnorm(tl, l, 0, ("XT", gi), t0, n, buf)
                hT = tl["hT"][buf]
                nw = 512 if cfg.get("na_wide", True) else n
                for qk in range(2):
                    dst = QT if qk == 0 else KT
                    for c in range(KD):
                        col = qk * D + c * 128
                        bank = 1 + (cnt % 2)
                        qb = cnt % 2
                        cnt += 1
                        for k in range(KD):
                            Tt(lambda e, k=k, col=col, bank=bank: e.matmul(PS[bank][:, :nw], lhsT=wq_b[:, k, col:col + 128], rhs=hT[:, k, :nw], start=(k == 0), stop=(k == KD - 1)),
                               [hk, "wq_b"], [("ps", bank)])
                        Aa(lambda e, bank=bank: e.activation(out=sqq[:, :nw], in_=PS[bank][:, :nw], func=AF.Square), [("ps", bank)], ["sqq"])
                        Tt(lambda e: e.matmul(PS[3][:, :nw], lhsT=bones_b[:], rhs=sqq[:, :nw], start=True, stop=True), ["sqq", "bones_b"], [("ps", 3)])
                        Aa(lambda e: e.activation(out=rsq[:, :nw], in_=PS[3][:, :nw], func=AF.Sqrt, scale=1.0 / 64, bias=eps_t[:, 0:1]), [("ps", 3), "eps"], ["rsq"])
                        Vv(lambda e: e.reciprocal(out=rsq[:, :nw], in_=rsq[:, :nw]), ["rsq"], ["rsq"])
                        Vv(lambda e, bank=bank, qb=qb, qk=qk: e.scalar_tensor_tensor(out=qn[qb][:, :nw], in0=PS[bank][:, :nw], scalar=gq[:, qk:qk + 1], in1=rsq[:, :nw],
                                                                                      op0=ALU.mult, op1=ALU.mult), [("ps", bank), "gq", "rsq"], [("qn", qb)])
                        P.dma("gpsimd", dst[c * 128:(c + 1) * 128, t0:t0 + n], qn[qb][:, :n], reads=[("qn", qb)], writes=[("QK", qk, c, gi)])
                for sub in range(n // 128):
                    vb = (gi * 4 + sub) % 2
                    for hf in range(2):
                        bank = 4 + hf
                        for k in range(KD):
                            Tt(lambda e, k=k, hf=hf, bank=bank, sub=sub: e.matmul(PS[bank][:, :], lhsT=hT[:, k, sub * 128:(sub + 1) * 128], rhs=wq_b[:, k, 2 * D + hf * 512:2 * D + (hf + 1) * 512],
                                                                                  start=(k == 0), stop=(k == KD - 1)), [hk, "wq_b"], [("ps", bank)])
                        Aa(lambda e, hf=hf, bank=bank, vb=vb: e.copy(out=va[vb][:, hf * 8:(hf + 1) * 8, 0:64], in_=PS[bank][:, :].rearrange("p (h w) -> p h w", w=64)),
                           [("ps", bank)], [("va", vb)])
                    tt = t0 + sub * 128
                    P.dma("gpsimd", VA[tt:tt + 128, :], va[vb][:].rearrange("p h w -> p (h w)"), reads=[("va", vb)], writes=[("VA", tt)])
            P.barrier()
            P.flush()
        if cfg.get("dbg", False):
            dq = nc.dram_tensor("dbgq", [D, T], BF16, kind="ExternalOutput").ap()
            dk = nc.dram_tensor("dbgk", [D, T], BF16, kind="ExternalOutput").ap()
            dv = nc.dram_tensor("dbgv", [T, 16 * 66], BF16, kind="ExternalOutput").ap()
            P.dma("sync", dq[:, :], QT[:, :])
            P.dma("sync", dk[:, :], KT[:, :])
            P.dma("sync", dv[:, :], VA[:, :])
            P.barrier()
            P.flush()
        if cfg.get("na_stop", 9) < 2:
            return
        with contextlib.ExitStack() as st:
            qTh = [sb("qTh%d" % i, [64, T], BF16, st=st) for i in range(2)]
            kTh = [sb("kTh%d" % i, [64, T], BF16, st=st) for i in range(2)]
            vh = [sb("vh%d" % i, [128, T // 128, 66], BF16, st=st) for i in range(2)]
            BP = [sb("BP%d" % i, [128, NPAT, 5, 64], st=st) for i in range(2)]
            sbias = sb("sbias", [128, 5 + NCC, 64], st=st)
            Ee = [sb("Ee%d" % i, [128, 5 + NCC, 64], BF16, st=st) for i in range(2)]
            o_sb = [sb("o_sb%d" % i, [64, ROWS, 64], BF16, st=st) for i in range(2)]
            rec = sb("rec", [128, 1], st=st)
            Ec = sb("Ec", [128, NCC, CTX], BF16, st=st)
            oc_sb = [sb("oc_sb%d" % i, [128, NCC, 64], BF16, st=st) for i in range(2)]
            for i in range(2):
                Vv(lambda e, i=i: e.memset(BP[i][:], NEGB), [], [("BP", i)])
            NK = 5 + NCC
            for h in cfg.get("na_hlist", list(range(16))):
                if cfg.get("na_bar", 0) >= 1:
                    P.barrier()
                if cfg.get("na_flush", True) and h != cfg.get("na_hlist", [0])[0]:
                    P.barrier()
                    P.flush()
                hb = (h % 2) if cfg.get("na_db", False) else 0
                hs = cfg.get("na_hsrc", h) if cfg.get("na_hsrc", -1) >= 0 else h
                msk_ = cfg.get("na_hmask", 15)
                hq_, hk_, hv_, hbb_ = [(hs if (msk_ >> i) & 1 else h) for i in range(4)]
                P.dma("sync", qTh[hb][:], QT[hq_ * 64:(hq_ + 1) * 64, :], writes=[("qTh", hb)])
                P.dma("sync", kTh[hb][:], KT[hk_ * 64:(hk_ + 1) * 64, :], writes=[("kTh", hb)])
                for cch in range(0, T // 128, 8):
                    ce = min(cch + 8, T // 128)
                    P.dma("scalar", vh[hb][:, cch:ce, :], VA[cch * 128:ce * 128, hv_ * 66:(hv_ + 1) * 66].rearrange("(c p) w -> p c w", p=128), writes=[("vh", hb)])
                for pi, (rr0, off) in enumerate(plist):
                    for hf in range(2):
                        ws = [w for w in range(8) if (off + w) % 2 == hf]
                        jj0 = (off + ws[0]) // 2
                        P.dma("scalar", BP[hb][hf * 64:(hf + 1) * 64, pi, jj0:jj0 + 4, :],
                              br_rel_in[j, hbb_, ((rr0 + ws[0]) // 2 if (rr0 + ws[0]) % 2 == 0 else 8 + (rr0 + ws[0]) // 2):((rr0 + ws[0]) // 2 if (rr0 + ws[0]) % 2 == 0 else 8 + (rr0 + ws[0]) // 2) + 4, :, :].rearrange("w k q -> k w q"), writes=[("BP", hb)])
                for r in range(ROWS):
                    if cfg.get("na_bar", 0) >= 2:
                        P.barrier()
                    c0, pi = rowinfo[r]
                    eb_ = (r % 2) if cfg.get("na_db", False) else 0
                    tq = CTX + r * 64
                    for jj in range(NK):
                        tk = CTX + (c0 + jj) * 128 if jj < 5 else (jj - 5) * 128
                        Tt(lambda e, jj=jj, tk=tk, tq=tq: e.matmul(PS[1 + eb_][:, jj * 64:(jj + 1) * 64], lhsT=kTh[hb][:, tk:tk + 128], rhs=qTh[hb][:, tq:tq + 64], start=True, stop=True),
                           [("kTh", hb), ("qTh", hb)], [("ps", 1 + eb_)])
                    Vv(lambda e, pi=pi: e.scalar_tensor_tensor(out=sbias[:, 0:5, :], in0=PS[1 + eb_][:, 0:320].rearrange("p (a q) -> p a q", q=64), scalar=0.125,
                                                               in1=BP[hb][:, pi, :, :], op0=ALU.mult, op1=ALU.add), [("ps", 1 + eb_), ("BP", hb)], ["sbias"])
                    Aa(lambda e: e.activation(out=Ee[eb_][:, 0:5, :], in_=sbias[:, 0:5, :], func=AF.Exp), ["sbias"], [("Ee", eb_)])
                    Aa(lambda e: e.activation(out=Ee[eb_][:, 5:NK, :], in_=PS[1 + eb_][:, 320:NK * 64].rearrange("p (a q) -> p a q", q=64), func=AF.Exp, scale=0.125),
                       [("ps", 1 + eb_)], [("Ee", eb_)])
                    for jj in range(NK):
                        ck = NCC + c0 + jj if jj < 5 else jj - 5
                        Tt(lambda e, jj=jj, ck=ck: e.matmul(PS[3 + eb_][:64, 0:65], lhsT=Ee[eb_][:, jj, :], rhs=vh[hb][:, ck, 0:65], start=(jj == 0), stop=(jj == NK - 1)),
                           [("Ee", eb_), ("vh", hb)], [("ps", 3 + eb_)])
                    Vv(lambda e: e.reciprocal(out=rec[:64, :], in_=PS[3 + eb_][:64, 64:65]), [("ps", 3 + eb_)], ["rec"])
                    Vv(lambda e, r=r: e.tensor_scalar(out=o_sb[hb][:, r, :], in0=PS[3 + eb_][:64, 0:64], scalar1=rec[:64, 0:1], scalar2=None, op0=ALU.mult),
                       [("ps", 3 + eb_), "rec"], [("o_sb", hb)])
                for r0 in range(0, ROWS, 16):
                    P.dma("gpsimd", ON[CTX + r0 * 64:CTX + (r0 + 16) * 64, h * 64:(h + 1) * 64].rearrange("(r q) d -> q r d", q=64), o_sb[hb][:, r0:r0 + 16, :],
                          reads=[("o_sb", hb)], writes=[("ON", h, r0)])
                for jc in range(NCC):
                    Tt(lambda e, jc=jc: e.matmul(PS[5][:, jc * CTX:(jc + 1) * CTX], lhsT=kTh[hb][:, jc * 128:(jc + 1) * 128], rhs=qTh[hb][:, 0:CTX], start=True, stop=True),
                       [("kTh", hb), ("qTh", hb)], [("ps", 5)])
                Aa(lambda e: e.activation(out=Ec[:], in_=PS[5][:, 0:NCC * CTX].rearrange("p (a q) -> p a q", q=CTX), func=AF.Exp, scale=0.125), [("ps", 5)], ["Ec"])
                for qc in range(NCC):
                    for jc in range(NCC):
                        Tt(lambda e, jc=jc, qc=qc: e.matmul(PS[6][:, 0:65], lhsT=Ec[:, jc, qc * 128:(qc + 1) * 128], rhs=vh[hb][:, jc, 0:65], start=(jc == 0), stop=(jc == NCC - 1)),
                           ["Ec", ("vh", hb)], [("ps", 6)])
                    Vv(lambda e: e.reciprocal(out=rec[:, :], in_=PS[6][:, 64:65]), [("ps", 6)], ["rec"])
                    Vv(lambda e, qc=qc: e.tensor_scalar(out=oc_sb[hb][:, qc, :], in0=PS[6][:, 0:64], scalar1=rec[:, 0:1], scalar2=None, op0=ALU.mult),
                       [("ps", 6), "rec"], [("oc_sb", hb)])
                P.dma("gpsimd", ON[0:CTX, h * 64:(h + 1) * 64].rearrange("(c p) d -> p c d", p=128), oc_sb[hb][:], reads=[("oc_sb", hb)], writes=[("ONc", h)])
            P.barrier()
            P.flush()
        if cfg.get("na_stop", 9) < 3:
            return
        with contextlib.ExitStack() as st:
            wst = sb("wst", [128, D], st=st)
            wo_b = sb("wo_b", [128, KD, D], BF16, st=st)
            for k in range(KD):
                load_cast(wst, wo_b[:, k, :], wno_in[j, k * 128:(k + 1) * 128, :], D, "wst", "wo_b")
            on_t = [sb("on_t%d" % i, [128, D], BF16, st=st) for i in range(2)]
            oT = sb("oT", [128, KD, 512], BF16, st=st)
            xg = [sb("xg%d" % i, [128, KD, 512], st=st) for i in range(2)]
            cnt = 0
            for gi, (t0, n) in enumerate(GS):
                isc = 0 if t0 >= CTX else 1
                buf = gi % 2
                nw = 512
                P.dma("sync", xg[buf][:, :, :n], XT[:, t0:t0 + n].rearrange("(k p) t -> p k t", p=128), writes=[("xg", buf)])
                for sub in range(n // 128):
                    ob = cnt % 2
                    cnt += 1
                    P.dma("scalar", on_t[ob][:], ON[t0 + sub * 128:t0 + (sub + 1) * 128, :], writes=[("on_t", ob)])
                    for a4 in range(0, KD, 4):
                        for a in range(a4, a4 + 4):
                            Tt(lambda e, a=a, a4=a4, ob=ob: e.transpose(PT[:, (a - a4) * 128:(a - a4 + 1) * 128], on_t[ob][:, a * 128:(a + 1) * 128], ident_b[:, :]),
                               [("on_t", ob), "ident_b"], ["PT"])
                        Vv(lambda e, sub=sub, a4=a4: e.tensor_copy(out=oT[:, a4:a4 + 4, sub * 128:(sub + 1) * 128], in_=PT[:, :].rearrange("p (a t) -> p a t", t=128)), ["PT"], ["oT"])
                for dc in range(KD):
                    bank = 1 + dc % 2
                    for k in range(KD):
                        Tt(lambda e, dc=dc, k=k, bank=bank: e.matmul(PS[bank][:, :nw], lhsT=wo_b[:, k, dc * 128:(dc + 1) * 128], rhs=oT[:, k, :nw], start=(k == 0), stop=(k == KD - 1)),
                           ["oT", "wo_b"], [("ps", bank)])
                    Vv(lambda e, dc=dc, bank=bank: e.scalar_tensor_tensor(out=xg[buf][:, dc, :nw], in0=PS[bank][:, :nw], scalar=MV[:, l, 16 + dc, isc:isc + 1], in1=xg[buf][:, dc, :nw],
                                                                          op0=ALU.mult, op1=ALU.add), [("ps", bank), ("xg", buf), "MV"], [("xg", buf)])
                P.dma("gpsimd", XT[:, t0:t0 + n].rearrange("(k p) t -> p k t", p=128), xg[buf][:, :, :n], reads=[("xg", buf)], writes=[("XT", gi)])
            P.barrier()
            P.flush()

    def moe_layer(l):
        with contextlib.ExitStack() as st:
            P.barrier()
            tl = mn_tiles(st, 512, 2)
            hF = sb("hF", [128, KD, 512], st=st)
            hm_ = sb("hm_", [128, KD, 512], st=st)
            wr_f = sb("wr_f", [128, KD, NE], st=st)
            br_t = sb("br_t", [128, NE], st=st)
            lg = sb("lg", [128, NE], st=st)
            mx8 = sb("mx8", [128, 8], st=st)
            msk = sb("msk", [128, NE], st=st)
            ex = sb("ex", [128, NE], st=st)
            ssum = sb("ssum", [128, 1], st=st)
            cbT = sb("cbT", [NE, 512], st=st)
            cbm = sb("cbm", [NE, 512], st=st)
            P.dma("sync", wr_f[:], wr_in[l].rearrange("(k p) e -> p k e", p=128), writes=["wr_f"])
            P.dma("sync", br_t[:], br_in[l:l + 1, :].partition_broadcast(128), writes=["br_t"])
            for gi, (t0, n) in enumerate(GS):
                hk = modnorm(tl, l, 1, ("XT", gi), t0, n, 0, out_f32=hF)
                for c in range(n // 128):
                    for k in range(KD):
                        Tt(lambda e, k=k, c=c: e.matmul(PS[1][:, :NE], lhsT=hF[:, k, c * 128:(c + 1) * 128], rhs=wr_f[:, k, :], start=(k == 0), stop=(k == KD - 1)),
                           [hk, "wr_f"], [("ps", 1)])
                    Vv(lambda e: e.tensor_tensor(out=lg[:], in0=PS[1][:, :NE], in1=br_t[:], op=ALU.add), [("ps", 1), "br_t"], ["lg"])
                    Vv(lambda e: e.max(out=mx8[:], in_=lg[:]), ["lg"], ["mx8"])
                    Vv(lambda e: e.tensor_scalar(out=msk[:], in0=lg[:], scalar1=mx8[:, 3:4], scalar2=None, op0=ALU.is_ge), ["lg", "mx8"], ["msk"])
                    Vv(lambda e: e.tensor_scalar(out=ex[:], in0=lg[:], scalar1=mx8[:, 0:1], scalar2=None, op0=ALU.subtract), ["lg", "mx8"], ["ex"])
                    Aa(lambda e: e.activation(out=ex[:], in_=ex[:], func=AF.Exp), ["ex"], ["ex"])
                    Vv(lambda e: e.tensor_tensor(out=ex[:], in0=ex[:], in1=msk[:], op=ALU.mult), ["ex", "msk"], ["ex"])
                    Vv(lambda e: e.reduce_sum(out=ssum[:], in_=ex[:], axis=AX.X), ["ex"], ["ssum"])
                    Vv(lambda e: e.reciprocal(out=ssum[:], in_=ssum[:]), ["ssum"], ["ssum"])
                    Vv(lambda e: e.tensor_scalar(out=ex[:], in0=ex[:], scalar1=ssum[:, 0:1], scalar2=None, op0=ALU.mult), ["ex", "ssum"], ["ex"])
                    Tt(lambda e, c=c: e.transpose(PS[2][:NE, c * 128:(c + 1) * 128], ex[:, :], ident_f[:, :]), ["ex", "ident_f"], [("ps", 2)])
                Vv(lambda e: e.tensor_copy(out=cbT[:, :n], in_=PS[2][:NE, :n]), [("ps", 2)], ["cbT"])
                for s in range(4):
                    Vv(lambda e, s=s: e.tensor_scalar(out=hm_[:, :, :n], in0=hF[:, :, :n], scalar1=mw4[:, s:s + 1], scalar2=None, op0=ALU.mult), [hk, "mw4"], ["hm_"])
                    P.dma("gpsimd", GIN[s * D:(s + 1) * D, t0:t0 + n].rearrange("(k p) t -> p k t", p=128), hm_[:, :, :n], reads=["hm_"], writes=["GIN"])
                    Vv(lambda e, s=s: e.tensor_scalar(out=cbm[:, :n], in0=cbT[:, :n], scalar1=mw4[:NE, s:s + 1], scalar2=None, op0=ALU.mult), ["cbT", "mw4"], ["cbm"])
                    P.dma("gpsimd", CBIN[:, s * T + t0:s * T + t0 + n], cbm[:, :n], reads=["cbm"], writes=["CBIN"])
            allreduce(GIN[:, :], GT[:, :], ["GIN"], ["GT"])
            allreduce(CBIN[:, :], CBF[:, :], ["CBIN"], ["CBF"])
            P.barrier()
            P.flush()
        with contextlib.ExitStack() as st:
            selE = sb("selE_s", [NE, EPC, 128], st=st)
            P.dma("sync", selE[:], selE_in[:, :, :], writes=["selE"])
            hin = [sb("hin%d" % i, [128, KD, 512], st=st) for i in range(2)]
            hb_ = [sb("hb_%d" % i, [128, KD, 512], BF16, st=st) for i in range(2)]
            cbt = [sb("cbt%d" % i, [NE, 512], st=st) for i in range(2)]
            wst = sb("wst", [128, 2 * DFF], st=st)
            wgu_b = sb("wgu_b", [128, KD, 2 * DFF], BF16, st=st)
            wd_b = sb("wd_b", [128, KF, D], BF16, st=st)
            bgu_t = sb("bgu_t", [128, 2 * KF], st=st)
            bd_t = sb("bd_t", [128, KD], st=st)
            cb = sb("cb", [128, 512], st=st)
            gt_ = sb("gt_", [128, 512], st=st)
            ut_ = sb("ut_", [128, 512], st=st)
            sg_ = sb("sg_", [128, 512], st=st)
            actT = sb("actT", [128, KF, 512], BF16, st=st)
            facc = sb("facc", [128, KD, 512], st=st)
            fprev = sb("fprev", [128, KD, 512], st=st)
            cnt = 0
            for j in range(EPC):
                for k in range(KD):
                    load_cast(wst, wgu_b[:, k, :], wgu_in[l, j, k * 128:(k + 1) * 128, :], 2 * DFF, "wst", "wgu_b")
                for k in range(KF):
                    load_cast(wst, wd_b[:, k, :], wd_in[l, j, k * 128:(k + 1) * 128, :], D, "wst", "wd_b")
                for c in range(2 * KF):
                    P.dma("sync", bgu_t[:, c:c + 1], bgu_in[l, j, c * 128:(c + 1) * 128].rearrange("(p o) -> p o", o=1), writes=["bgu_t"])
                for c in range(KD):
                    P.dma("sync", bd_t[:, c:c + 1], bd_in[l, j, c * 128:(c + 1) * 128].rearrange("(p o) -> p o", o=1), writes=["bd_t"])
                for s in range(4):
                    for gi, (t0, n) in enumerate(GS):
                        buf = cnt % 2
                        cnt += 1
                        hk = ("hb_", buf)
                        P.dma("sync", hin[buf][:, :, :n], GT[s * D:(s + 1) * D, t0:t0 + n].rearrange("(k p) t -> p k t", p=128), reads=["GT"], writes=[("hin", buf)])
                        Gg(lambda e, buf=buf: e.tensor_copy(out=hb_[buf][:, :, :n], in_=hin[buf][:, :, :n]), [("hin", buf)], [hk])
                        P.dma("scalar", cbt[buf][:, :n], CBF[:, s * T + t0:s * T + t0 + n], reads=["CBF"], writes=[("cbt", buf)])
                        Tt(lambda e, buf=buf, j=j: e.matmul(PS[0][:, :n], lhsT=selE[:, j, :], rhs=cbt[buf][:, :n], start=True, stop=True), [("cbt", buf), "selE"], [("ps", 0)])
                        Aa(lambda e: e.copy(out=cb[:, :n], in_=PS[0][:, :n]), [("ps", 0)], ["cb"])
                        for i in range(KF):
                            pg, pu = 3 + (i % 2) * 2, 4 + (i % 2) * 2
                            for k in range(KD):
                                Tt(lambda e, k=k, i=i, pg=pg, buf=buf: e.matmul(PS[pg][:, :n], lhsT=wgu_b[:, k, i * 128:(i + 1) * 128], rhs=hb_[buf][:, k, :n],
                                                                                start=(k == 0), stop=(k == KD - 1)), [hk, "wgu_b"], [("ps", pg)])
                            for k in range(KD):
                                Tt(lambda e, k=k, i=i, pu=pu, buf=buf: e.matmul(PS[pu][:, :n], lhsT=wgu_b[:, k, DFF + i * 128:DFF + (i + 1) * 128], rhs=hb_[buf][:, k, :n],
                                                                                start=(k == 0), stop=(k == KD - 1)), [hk, "wgu_b"], [("ps", pu)])
                            Vv(lambda e, i=i, pg=pg: e.tensor_scalar(out=gt_[:, :n], in0=PS[pg][:, :n], scalar1=bgu_t[:, i:i + 1], scalar2=7.0, op0=ALU.add, op1=ALU.min),
                               [("ps", pg), "bgu_t"], ["gt_"])
                            Vv(lambda e, i=i, pu=pu: e.tensor_scalar(out=ut_[:, :n], in0=PS[pu][:, :n], scalar1=bgu_t[:, KF + i:KF + i + 1], scalar2=7.0, op0=ALU.add, op1=ALU.min),
                               [("ps", pu), "bgu_t"], ["ut_"])
                            Gg(lambda e: e.tensor_scalar(out=ut_[:, :n], in0=ut_[:, :n], scalar1=-7.0, scalar2=1.0, op0=ALU.max, op1=ALU.add), ["ut_"], ["ut_"])
                            Aa(lambda e: e.activation(out=sg_[:, :n], in_=gt_[:, :n], func=AF.Sigmoid, scale=1.702), ["gt_"], ["sg_"])
                            Gg(lambda e: e.tensor_tensor(out=ut_[:, :n], in0=ut_[:, :n], in1=cb[:, :n], op=ALU.mult), ["ut_", "cb"], ["ut_"])
                            Vv(lambda e: e.tensor_tensor(out=gt_[:, :n], in0=gt_[:, :n], in1=sg_[:, :n], op=ALU.mult), ["gt_", "sg_"], ["gt_"])
                            Vv(lambda e, i=i: e.tensor_tensor(out=actT[:, i, :n], in0=gt_[:, :n], in1=ut_[:, :n], op=ALU.mult), ["gt_", "ut_"], ["actT"])
                        if j > 0:
                            P.dma("scalar", fprev[:, :, :n], FT[s * D:(s + 1) * D, t0:t0 + n].rearrange("(k p) t -> p k t", p=128), reads=[("FT", s, gi)], writes=["fprev"])
                        for dc in range(KD):
                            pd = 1 + (dc % 2)
                            for i in range(KF):
                                Tt(lambda e, i=i, dc=dc, pd=pd: e.matmul(PS[pd][:, :n], lhsT=wd_b[:, i, dc * 128:(dc + 1) * 128], rhs=actT[:, i, :n],
                                                                         start=(i == 0), stop=(i == KF - 1)), ["actT", "wd_b"], [("ps", pd)])
                            Vv(lambda e, dc=dc, pd=pd: e.scalar_tensor_tensor(out=facc[:, dc, :n], in0=cb[:, :n], scalar=bd_t[:, dc:dc + 1], in1=PS[pd][:, :n], op0=ALU.mult, op1=ALU.add),
                               [("ps", pd), "cb", "bd_t"], ["facc"])
                        if j > 0:
                            Gg(lambda e: e.tensor_tensor(out=facc[:, :, :n], in0=facc[:, :, :n], in1=fprev[:, :, :n], op=ALU.add), ["facc", "fprev"], ["facc"])
                        P.dma("gpsimd", FT[s * D:(s + 1) * D, t0:t0 + n].rearrange("(k p) t -> p k t", p=128), facc[:, :, :n], reads=["facc"], writes=[("FT", s, gi), "FTall"])
            allreduce(FT[:, :], FR[:, :], ["FTall"], ["FR"])
            P.barrier()
            P.flush()
        with contextlib.ExitStack() as st:
            f4 = sb("f4", [128, 4, 512], st=st)
            fm = sb("fm", [128, 512], st=st)
            xo = sb("xo", [128, 512], st=st)
            for gi, (t0, n) in enumerate(GS):
                isc = 0 if t0 >= CTX else 1
                for k in range(KD):
                    P.dma("sync", f4[:, :, :n], FR.rearrange("(s k p) t -> k p s t", s=4, p=128)[k][:, :, t0:t0 + n], reads=["FR"], writes=["f4"])
                    P.dma("scalar", xo[:, :n], XT[k * 128:(k + 1) * 128, t0:t0 + n], reads=[("XT", gi)], writes=["xo"])
                    Vv(lambda e: e.tensor_scalar(out=fm[:, :n], in0=f4[:, 0, :n], scalar1=mr4[:, 0:1], scalar2=None, op0=ALU.mult), ["f4", "mr4"], ["fm"])
                    for s in range(1, 4):
                        Vv(lambda e, s=s: e.scalar_tensor_tensor(out=fm[:, :n], in0=f4[:, s, :n], scalar=mr4[:, s:s + 1], in1=fm[:, :n], op0=ALU.mult, op1=ALU.add),
                           ["f4", "fm"], ["fm"])
                    Vv(lambda e, k=k, isc=isc: e.scalar_tensor_tensor(out=xo[:, :n], in0=fm[:, :n], scalar=MV[:, l, 40 + k, isc:isc + 1], in1=xo[:, :n], op0=ALU.mult, op1=ALU.add),
                       ["fm", "xo", "MV"], ["xo"])
                    P.dma("gpsimd", XT[k * 128:(k + 1) * 128, t0:t0 + n], xo[:, :n], reads=["xo"], writes=[("XT", gi)])
            P.barrier()
            P.flush()

    for l in range(DEPTH):
        if l % 2 == 0 and "r" in mixers:
            rec_layer(l)
        if l % 2 == 1 and "n" in mixers:
            na_layer(l)
        if cfg.get("moe", True):
            moe_layer(l)

    with contextlib.ExitStack() as st:
        P.barrier()
        oh = [sb("oh%d" % i, [128, KD, 512], st=st) for i in range(2)]
        for i, t0 in enumerate(range(0, SEQ, 512)):
            b = i % 2
            P.dma("sync", oh[b][:], XT[:, CTX + t0:CTX + t0 + 512].rearrange("(k p) t -> p k t", p=128), writes=[("oh", b)])
            P.dma("gpsimd", out_ext[:, t0:t0 + 512].rearrange("(k p) t -> p k t", p=128), oh[b][:], reads=[("oh", b)], writes=["OUT"])
        P.barrier()
        P.flush()
    stack.close()
    return nc


CFG_FULL = dict(SEQ=8192, CTX=256, DEPTH=4, NE=32, DFF=1024)


def scan_consts():
    s = np.arange(64)[:, None]
    t = np.arange(64)[None, :]
    sc = np.zeros((64, 580), np.float32)
    sc[:, 0:64] = (s <= t).astype(np.float32) - (s <= 31).astype(np.float32)
    sc[:, 64:128] = (s >= t).astype(np.float32) - (s >= 32).astype(np.float32)
    sc[:, 128:192] = (s <= t)
    sc[:, 192:256] = (s >= t)
    sc[:, 256:320] = (s <= t)
    sc[:, 320:384] = (s >= t)
    sc[:, 384:448] = 1.0
    sc[:, 448] = (s[:, 0] <= 31)
    sc[:, 449] = (s[:, 0] > 31)
    sc[:, 450] = (s[:, 0] >= 32)
    sc[:, 451] = (s[:, 0] < 32)
    return sc


def make_inputs(cfg, inp):
    SEQ, CTX, DEPTH, NE, DFF = cfg["SEQ"], cfg["CTX"], cfg["DEPTH"], cfg["NE"], cfg["DFF"]
    T = SEQ + CTX
    EPC = NE // 8
    NEVEN, NODD = (DEPTH + 1) // 2, DEPTH // 2
    f = lambda a: np.ascontiguousarray(a, dtype=np.float32)
    cT = f(np.concatenate([inp["c"], inp["c_ctx"][None]], 0).T)
    o = np.concatenate([[0], np.cumsum((512, 512, 512, 512, 512, 256, 256, 512, 512, 4, 4, 4, 4))])
    rng = lambda i: np.arange(o[i], o[i + 1])
    p64 = np.concatenate([np.arange(16, 32), np.arange(0, 16), np.arange(48, 64), np.arange(32, 48)])
    sw = np.concatenate([h * 64 + p64 for h in range(4)])
    perm = np.concatenate([rng(0), rng(3), rng(5), rng(5)[sw], rng(6), rng(6)[sw], rng(7), rng(1), rng(2), rng(4), rng(8),
                           rng(9), rng(10), rng(11), rng(12)])
    assert perm.size == NCW
    w2 = f(inp["rec_w_in"][:, :, perm])
    b2 = f(inp["rec_b_in"][:, perm])
    pos = np.arange(SEQ)
    inv = (10000.0 ** (-np.arange(16, dtype=np.float32) / 16)).astype(np.float32)
    ar = (pos // 64).astype(np.float32)[:, None] * inv[None, :]
    ac = (pos % 64).astype(np.float32)[:, None] * inv[None, :]
    ropec = np.ones((T, 64), np.float32)
    ropes = np.zeros((T, 64), np.float32)
    ropec[CTX:] = np.concatenate([np.cos(ar), np.cos(ar), np.cos(ac), np.cos(ac)], 1)
    ropes[CTX:] = np.concatenate([-np.sin(ar), np.sin(ar), -np.sin(ac), np.sin(ac)], 1)
    cpos = np.arange(64)
    cstart = np.clip(cpos - 8, 0, 48)
    col_ok = (cpos[None, :] >= cstart[:, None]) & (cpos[None, :] < cstart[:, None] + 16)
    rel_c = np.clip(cpos[None, :] - cpos[:, None], -15, 15) + 15
    if NODD:
        rpb = f(inp["na_rpb"])
        bq = rpb[:, :, :, rel_c]
        bq = np.where(col_ok[None, None, None], bq, np.float32(NEGB))
        bt_ = bq.transpose(0, 1, 2, 4, 3)
        brel = f(np.concatenate([bt_[:, :, 0::2], bt_[:, :, 1::2], bt_[:, :, 0:1]], axis=2))
        wqkv = f(inp["na_w_qkv"]); wno = f(inp["na_w_out"])
        qg = f(np.tile(inp["na_q_g"], (1, 2))); kg = f(np.tile(inp["na_k_g"], (1, 2)))
    else:
        brel = np.zeros((1, 16, 16, 64, 64), np.float32)
        wqkv = np.zeros((1, D, 3 * D), np.float32); wno = np.zeros((1, D, D), np.float32)
        qg = np.zeros((1, 128), np.float32); kg = np.zeros((1, 128), np.float32)
    bones = np.zeros((128, 128), np.float32)
    bones[:64, :64] = 1
    bones[64:, 64:] = 1
    scanc = scan_consts()
    maps = []
    for c in range(8):
        b = c % 4
        m = {}
        m["xT"] = f(np.concatenate([inp["ctx"][b], inp["x"][b]], 0).T)
        m["cT"] = cT
        m["adaw"] = f(inp["ada_w"][:, :, c * 768:(c + 1) * 768])
        m["adab"] = f(inp["ada_b"][:, c * 768:(c + 1) * 768])
        m["gmix"] = f(inp["norm_mix_g"])
        m["gffn"] = f(inp["norm_ffn_g"])
        m8 = np.zeros((5, 8), np.float32); m8[:, c] = 1
        m["m8"] = m8
        sel = np.zeros((5, 2), np.float32); sel[b, 0] = 1; sel[4, 1] = 1
        m["sel"] = sel
        mw = np.zeros((128, 4), np.float32)
        if c < 4:
            mw[:, b] = 1
        m["mw4"] = mw
        mr = np.zeros((128, 4), np.float32); mr[:, b] = 1
        m["mr4"] = mr
        se = np.zeros((NE, EPC, 128), np.float32)
        for jj in range(EPC):
            se[c * EPC + jj, jj, :] = 1
        m["selE"] = se
        m["ident"] = np.eye(128, dtype=np.float32)
        m["wr"] = f(inp["moe_w_router"])
        m["br"] = f(inp["moe_b_router"])
        m["wgu"] = f(inp["moe_w_gu"][:, c * EPC:(c + 1) * EPC])
        m["bgu"] = f(inp["moe_b_gu"][:, c * EPC:(c + 1) * EPC])
        m["wd"] = f(inp["moe_w_down"][:, c * EPC:(c + 1) * EPC])
        m["bd"] = f(inp["moe_b_down"][:, c * EPC:(c + 1) * EPC])
        m["w2"] = w2
        m["b2"] = b2
        m["wro"] = f(inp["rec_w_out"])
        m["lbr"] = f(inp["hgrn_lb"])
        m["ghg"] = f(inp["hgrn_out_g"])
        m["gml"] = f(inp["mlstm_out_g"])
        m["ropec"] = ropec
        m["ropes"] = ropes
        m["scanc"] = scanc
        m["wqkv"] = wqkv
        m["wno"] = wno
        m["qg"] = qg
        m["kg"] = kg
        m["brel"] = brel
        m["bones"] = bones
        maps.append(m)
    return maps


def run(cfg, inp):
    nc = build(cfg)
    maps = make_inputs(cfg, inp)
    res = run_bass_kernel_spmd(nc, maps, core_ids=list(range(8)))
    if cfg.get("dbg", False):
        for nm in ("dbgq", "dbgk", "dbgv"):
            a = np.asarray(res.results[0][nm]).astype(np.float32)
            print(nm, a.shape, "nan", np.isnan(a).sum(), "inf", np.isinf(a).sum(), "absmax", np.nanmax(np.abs(a)))
            if nm != "dbgv":
                print("  per-head rms", [float(np.sqrt(np.mean(a[h * 64:(h + 1) * 64] ** 2))) for h in range(4)])
    SEQ = cfg["SEQ"]
    out = np.zeros((4, SEQ, D), np.float32)
    for b in range(4):
        out[b] = res.results[b]["out"].T
    return out


def kernel(**inputs):
    return run(CFG_FULL, {k: np.asarray(v) for k, v in inputs.items()})
```
